# Optimizing a Trainium2 kernel written in Bass

```python
import math
import jax, jax.numpy as jnp
from jax import lax
import numpy as np

D_MODEL = 4096
BATCH = 1
SEQ = 16384
DEPTH = 4

GRID_W = 64
CTX_LEN = 256
HEAD_DIM = 128
BLOCK = 128
WINDOW = 128
ROPE_THETA = 10000.0
EPS = 1e-6
ADALN_RANK = 512
N_MOD = 6

A_HEADS = D_MODEL // (2 * HEAD_DIM)
A_KV_HEADS = A_HEADS // 4
B_HEADS = D_MODEL // (4 * HEAD_DIM)
B_KV_HEADS = B_HEADS // 2
B_DV = 2 * HEAD_DIM
C_HEADS = D_MODEL // (2 * HEAD_DIM)
C_KV_HEADS = C_HEADS // 4
D_HEADS = D_MODEL // (2 * HEAD_DIM)
D_Q_RANK = 1536
D_KV_RANK = 512
D_NOPE = 128
D_ROPE = 64
D_V = 128
D_QK = D_NOPE + D_ROPE
N_EXPERTS = 32
TOP_K = 4
EXPERT_FF = 384
SWIGLU_ALPHA = 1.702
SWIGLU_LIMIT = 7.0

EV_Q_SIZES = (A_HEADS * HEAD_DIM, B_HEADS * 2 * HEAD_DIM)
EV_KV_SIZES = (A_KV_HEADS * HEAD_DIM, A_KV_HEADS * HEAD_DIM, B_KV_HEADS * 2 * HEAD_DIM, B_KV_HEADS * B_DV)
EV_Q = sum(EV_Q_SIZES)
EV_IN = EV_Q + sum(EV_KV_SIZES)
EV_OUT = A_HEADS * HEAD_DIM + B_HEADS * B_DV
OD_Q_SIZES = (C_HEADS * HEAD_DIM, D_Q_RANK)
OD_KV_SIZES = (C_KV_HEADS * HEAD_DIM, C_KV_HEADS * HEAD_DIM, D_KV_RANK, D_ROPE)
OD_Q = sum(OD_Q_SIZES)
OD_IN = OD_Q + sum(OD_KV_SIZES)
OD_OUT = C_HEADS * HEAD_DIM + D_HEADS * D_V

kernel_name = 'hybrid_dit_swa_diff_axial_mla_moe'


def rms_norm(x, gain):
    xf = x.astype(jnp.float32)
    y = xf * lax.rsqrt(jnp.mean(xf * xf, axis=-1, keepdims=True) + EPS)
    return (y * gain.astype(jnp.float32)).astype(x.dtype)


def split_cols(p, sizes):
    out, start = [], 0
    for s in sizes:
        out.append(p[..., start:start + s])
        start += s
    return out


def axial_rope_tables(rows, dim):
    quarter = dim // 4
    inv_freq = ROPE_THETA ** (-jnp.arange(quarter, dtype=jnp.float32) / quarter)
    row = jnp.repeat(jnp.arange(rows, dtype=jnp.float32), GRID_W)
    col = jnp.tile(jnp.arange(GRID_W, dtype=jnp.float32), rows)
    ang = jnp.stack([row[:, None] * inv_freq, col[:, None] * inv_freq], axis=1)
    ang = jnp.broadcast_to(ang[:, :, None, :], (rows * GRID_W, 2, 2, quarter)).reshape(rows * GRID_W, dim)
    return jnp.cos(ang), jnp.sin(ang)


def rotate_axial_half(x):
    d = x.shape[-1]
    xs = x.reshape(x.shape[:-1] + (2, 2, d // 4))
    return jnp.stack([-xs[..., 1, :], xs[..., 0, :]], axis=-2).reshape(x.shape)


def apply_rope(x, cos, sin):
    shape = (1, x.shape[1]) + (1,) * (x.ndim - 3) + (x.shape[-1],)
    return x * cos.reshape(shape).astype(x.dtype) + rotate_axial_half(x) * sin.reshape(shape).astype(x.dtype)


def rope_tail(x, cos, sin, n_plain):
    return jnp.concatenate([x[..., :n_plain], apply_rope(x[..., n_plain:], cos, sin)], axis=-1)


def query_blocks(q):
    b, s = q.shape[:2]
    return jnp.moveaxis(q.reshape((b, s // BLOCK, BLOCK) + q.shape[2:]), 1, 0)


def unblock(o):
    nb, b, blk = o.shape[:3]
    return jnp.moveaxis(o, 0, 1).reshape((b, nb * blk) + o.shape[3:])


def blocked_attention(q, k, v, scale, sink=None):
    hk, g = q.shape[2], q.shape[3]

    def one_block(qi):
        s = jnp.einsum('bqhgd,bthd->bhgqt', qi, k, preferred_element_type=jnp.float32) * scale
        if sink is not None:
            sl = jnp.broadcast_to(sink.reshape(hk, g, 1, 1).astype(jnp.float32), s.shape[:-1] + (1,))
            p = jax.nn.softmax(jnp.concatenate([s, sl], axis=-1), axis=-1)[..., :-1]
        else:
            p = jax.nn.softmax(s, axis=-1)
        return jnp.einsum('bhgqt,bthe->bqhge', p.astype(v.dtype), v)

    return unblock(lax.map(one_block, query_blocks(q)))


def window_sink_attention(q, k, v, k_ctx, v_ctx, sink, scale):
    n, hk, g = q.shape[1], q.shape[2], q.shape[3]
    pad = ((0, 0), (BLOCK, BLOCK), (0, 0), (0, 0))
    kp = jnp.pad(k, pad)
    vp = jnp.pad(v, pad)
    n_loc = 3 * BLOCK
    n_ctx = k_ctx.shape[1]
    offs = jnp.arange(n_loc)[None, :] - BLOCK - jnp.arange(BLOCK)[:, None]
    in_window = jnp.abs(offs) <= WINDOW

    def one_block(args):
        i, qi = args
        kw = lax.dynamic_slice_in_dim(kp, i * BLOCK, n_loc, axis=1)
        vw = lax.dynamic_slice_in_dim(vp, i * BLOCK, n_loc, axis=1)
        kpos = (i - 1) * BLOCK + jnp.arange(n_loc)
        valid = in_window & ((kpos >= 0) & (kpos < n))[None, :]
        s_loc = jnp.einsum('bqhgd,bjhd->bhgqj', qi, kw, preferred_element_type=jnp.float32) * scale
        s_loc = jnp.where(valid, s_loc, -jnp.inf)
        s_ctx = jnp.einsum('bqhgd,bchd->bhgqc', qi, k_ctx, preferred_element_type=jnp.float32) * scale
        sl = jnp.broadcast_to(sink.reshape(hk, g, 1, 1).astype(jnp.float32), s_ctx.shape[:-1] + (1,))
        p = jax.nn.softmax(jnp.concatenate([s_loc, s_ctx, sl], axis=-1), axis=-1).astype(v.dtype)
        return (jnp.einsum('bhgqj,bjhe->bqhge', p[..., :n_loc], vw)
                + jnp.einsum('bhgqc,bche->bqhge', p[..., n_loc:n_loc + n_ctx], v_ctx))

    return unblock(lax.map(one_block, (jnp.arange(n // BLOCK), query_blocks(q))))


def diff_attention(q, k, v, lam, scale):
    def one_block(qi):
        s = jnp.einsum('bqhgcd,bthcd->bhgcqt', qi, k, preferred_element_type=jnp.float32) * scale
        p = jax.nn.softmax(s, axis=-1)
        a = p[:, :, :, 0] - lam * p[:, :, :, 1]
        return jnp.einsum('bhgqt,bthe->bqhge', a.astype(v.dtype), v)

    return unblock(lax.map(one_block, query_blocks(q)))


def mixer_window_diff(hc, hl, cos, sin, w_in, w_out, a_qn, a_kn, a_sink, b_qn, b_kn, b_lam, b_subln, lam_init, need_ctx):
    bsz = hl.shape[0]
    scale = HEAD_DIM ** -0.5
    lf = b_lam.astype(jnp.float32)
    lam = jnp.exp(jnp.sum(lf[0] * lf[1])) - jnp.exp(jnp.sum(lf[2] * lf[3])) + lam_init

    def shape_q(pq):
        n = pq.shape[1]
        qa, qb = split_cols(pq, EV_Q_SIZES)
        qa = rms_norm(qa.reshape(bsz, n, A_KV_HEADS, A_HEADS // A_KV_HEADS, HEAD_DIM), a_qn)
        qb = rms_norm(qb.reshape(bsz, n, B_KV_HEADS, B_HEADS // B_KV_HEADS, 2, HEAD_DIM), b_qn)
        return qa, qb

    def shape_kv(pkv):
        n = pkv.shape[1]
        ka, va, kb, vb = split_cols(pkv, EV_KV_SIZES)
        ka = rms_norm(ka.reshape(bsz, n, A_KV_HEADS, HEAD_DIM), a_kn)
        va = va.reshape(bsz, n, A_KV_HEADS, HEAD_DIM)
        kb = rms_norm(kb.reshape(bsz, n, B_KV_HEADS, 2, HEAD_DIM), b_kn)
        vb = vb.reshape(bsz, n, B_KV_HEADS, B_DV)
        return ka, va, kb, vb

    def merge(ya, yb):
        n = ya.shape[1]
        yb = rms_norm(yb.reshape(bsz, n, B_HEADS, B_DV), b_subln) * (1.0 - lam_init)
        return jnp.concatenate([ya.reshape(bsz, n, -1), yb.reshape(bsz, n, -1)], axis=-1) @ w_out

    ka_c, va_c, kb_c, vb_c = shape_kv(hc @ w_in[:, EV_Q:])
    p_l = hl @ w_in
    qa_l, qb_l = shape_q(p_l[..., :EV_Q])
    ka_l, va_l, kb_l, vb_l = shape_kv(p_l[..., EV_Q:])
    qa_l, ka_l = apply_rope(qa_l, cos, sin), apply_rope(ka_l, cos, sin)
    qb_l, kb_l = apply_rope(qb_l, cos, sin), apply_rope(kb_l, cos, sin)
    ya_l = window_sink_attention(qa_l, ka_l, va_l, ka_c, va_c, a_sink, scale)
    yb_l = diff_attention(qb_l, jnp.concatenate([kb_c, kb_l], axis=1), jnp.concatenate([vb_c, vb_l], axis=1), lam, scale)
    y_l = merge(ya_l, yb_l)
    if not need_ctx:
        return None, y_l
    qa_c, qb_c = shape_q(hc @ w_in[:, :EV_Q])
    y_c = merge(blocked_attention(qa_c, ka_c, va_c, scale, sink=a_sink), diff_attention(qb_c, kb_c, vb_c, lam, scale))
    return y_c, y_l


def mixer_axial_mla(hc, hl, cos, sin, cos_r, sin_r, w_in, w_out, c_qn, c_kn, d_qa_norm, d_kva_norm,
                    d_wq_up, d_wkv_up, d_qn, d_kn, need_ctx):
    bsz = hl.shape[0]

    def shape_q(pq, rope):
        n = pq.shape[1]
        qc, qd_a = split_cols(pq, OD_Q_SIZES)
        qc = rms_norm(qc.reshape(bsz, n, C_KV_HEADS, C_HEADS // C_KV_HEADS, HEAD_DIM), c_qn)
        qd = rms_norm((rms_norm(qd_a, d_qa_norm) @ d_wq_up).reshape(bsz, n, D_HEADS, 1, D_QK), d_qn)
        if rope:
            qc = apply_rope(qc, cos, sin)
            qd = rope_tail(qd, cos_r, sin_r, D_NOPE)
        return qc, qd

    def shape_kv(pkv, rope):
        n = pkv.shape[1]
        kc, vc, kv_a, kr = split_cols(pkv, OD_KV_SIZES)
        kc = rms_norm(kc.reshape(bsz, n, C_KV_HEADS, HEAD_DIM), c_kn)
        vc = vc.reshape(bsz, n, C_KV_HEADS, HEAD_DIM)
        kv = (rms_norm(kv_a, d_kva_norm) @ d_wkv_up).reshape(bsz, n, D_HEADS, D_NOPE + D_V)
        kr = jnp.broadcast_to(kr[:, :, None, :], (bsz, n, D_HEADS, D_ROPE))
        kd = rms_norm(jnp.concatenate([kv[..., :D_NOPE], kr], axis=-1), d_kn)
        vd = kv[..., D_NOPE:]
        if rope:
            kc = apply_rope(kc, cos, sin)
            kd = rope_tail(kd, cos_r, sin_r, D_NOPE)
        return kc, vc, kd, vd

    def merge(yc, yd):
        n = yc.shape[1]
        return jnp.concatenate([yc.reshape(bsz, n, -1), yd.reshape(bsz, n, -1)], axis=-1) @ w_out

    kc_c, vc_c, kd_c, vd_c = shape_kv(hc @ w_in[:, OD_Q:], False)
    p_l = hl @ w_in
    qc_l, qd_l = shape_q(p_l[..., :OD_Q], True)
    kc_l, vc_l, kd_l, vd_l = shape_kv(p_l[..., OD_Q:], True)
    cat = lambda a, b: jnp.concatenate([a, b], axis=1)
    y_l = merge(blocked_attention(qc_l, cat(kc_c, kc_l), cat(vc_c, vc_l), HEAD_DIM ** -0.5),
                blocked_attention(qd_l, cat(kd_c, kd_l), cat(vd_c, vd_l), D_QK ** -0.5))
    if not need_ctx:
        return None, y_l
    qc_c, qd_c = shape_q(hc @ w_in[:, :OD_Q], False)
    y_c = merge(blocked_attention(qc_c, kc_c, vc_c, HEAD_DIM ** -0.5),
                blocked_attention(qd_c, kd_c, vd_c, D_QK ** -0.5))
    return y_c, y_l


def moe_ffn(h, router_w, router_b, w_gu, b_gu, w_down, b_down):
    shp = h.shape
    t = h.reshape(-1, shp[-1])
    logits = jnp.dot(t, router_w, preferred_element_type=jnp.float32) + router_b.astype(jnp.float32)
    top_val, top_idx = lax.top_k(logits, TOP_K)
    top_w = jax.nn.softmax(top_val, axis=-1)
    gates = jnp.einsum('nk,nke->ne', top_w, jax.nn.one_hot(top_idx, N_EXPERTS, dtype=jnp.float32)).astype(t.dtype)
    out = jnp.zeros_like(t)
    for e in range(N_EXPERTS):
        gu = t @ w_gu[e] + b_gu[e]
        glu = jnp.minimum(gu[:, :EXPERT_FF], SWIGLU_LIMIT)
        lin = jnp.clip(gu[:, EXPERT_FF:], -SWIGLU_LIMIT, SWIGLU_LIMIT)
        act = glu * jax.nn.sigmoid(SWIGLU_ALPHA * glu) * (lin + 1.0)
        out = out + gates[:, e:e + 1] * (act @ w_down[e] + b_down[e])
    return out.reshape(shp)


def adaln(cvec, down, up, bias):
    return (jax.nn.silu(cvec) @ down) @ up + bias


def modulate(t, gain, shift, scale):
    return rms_norm(t, gain) * (1.0 + scale) + shift


def setup_inputs(seed: int = 0) -> dict:
    key = jax.random.key(seed)
    ks = iter(jax.random.split(key, 40))
    f32 = jnp.float32
    D = D_MODEL
    n_ev = (DEPTH + 1) // 2
    n_od = DEPTH // 2

    def nrm(shape, scale):
        return jax.random.normal(next(ks), shape, f32) * scale

    def gain(shape):
        return 1.0 + 0.02 * jax.random.normal(next(ks), shape, f32)

    return {
        'x': nrm((BATCH, SEQ, D), 1.0),
        'c': nrm((BATCH, D), 1.0),
        'ctx': nrm((BATCH, CTX_LEN, D), 1.0),
        'c_ctx': nrm((D,), 1.0),
        'adaln_down': nrm((DEPTH, D, ADALN_RANK), D ** -0.5),
        'adaln_up': nrm((DEPTH, ADALN_RANK, N_MOD * D), 0.5 * ADALN_RANK ** -0.5),
        'adaln_b': nrm((DEPTH, N_MOD * D), 0.01),
        'norm_mix': gain((DEPTH, D)),
        'norm_ffn': gain((DEPTH, D)),
        'ev_w_in': nrm((n_ev, D, EV_IN), D ** -0.5),
        'ev_w_out': nrm((n_ev, EV_OUT, D), EV_OUT ** -0.5),
        'ev_a_qn': gain((n_ev, HEAD_DIM)),
        'ev_a_kn': gain((n_ev, HEAD_DIM)),
        'ev_a_sink': nrm((n_ev, A_HEADS), 0.5),
        'ev_b_qn': gain((n_ev, HEAD_DIM)),
        'ev_b_kn': gain((n_ev, HEAD_DIM)),
        'ev_b_lam': nrm((n_ev, 4, HEAD_DIM), 0.1),
        'ev_b_subln': gain((n_ev, B_DV)),
        'od_w_in': nrm((n_od, D, OD_IN), D ** -0.5),
        'od_w_out': nrm((n_od, OD_OUT, D), OD_OUT ** -0.5),
        'od_c_qn': gain((n_od, HEAD_DIM)),
        'od_c_kn': gain((n_od, HEAD_DIM)),
        'od_d_qa_norm': gain((n_od, D_Q_RANK)),
        'od_d_kva_norm': gain((n_od, D_KV_RANK)),
        'od_d_wq_up': nrm((n_od, D_Q_RANK, D_HEADS * D_QK), D_Q_RANK ** -0.5),
        'od_d_wkv_up': nrm((n_od, D_KV_RANK, D_HEADS * (D_NOPE + D_V)), D_KV_RANK ** -0.5),
        'od_d_qn': gain((n_od, D_QK)),
        'od_d_kn': gain((n_od, D_QK)),
        'router_w': nrm((DEPTH, D, N_EXPERTS), D ** -0.5),
        'router_b': nrm((DEPTH, N_EXPERTS), 0.01),
        'moe_w_gu': nrm((DEPTH, N_EXPERTS, D, 2 * EXPERT_FF), D ** -0.5),
        'moe_b_gu': nrm((DEPTH, N_EXPERTS, 2 * EXPERT_FF), 0.01),
        'moe_w_down': nrm((DEPTH, N_EXPERTS, EXPERT_FF, D), EXPERT_FF ** -0.5),
        'moe_b_down': nrm((DEPTH, N_EXPERTS, D), 0.01),
    }


def reference(x, c, ctx, c_ctx, adaln_down, adaln_up, adaln_b, norm_mix, norm_ffn,
              ev_w_in, ev_w_out, ev_a_qn, ev_a_kn, ev_a_sink, ev_b_qn, ev_b_kn, ev_b_lam, ev_b_subln,
              od_w_in, od_w_out, od_c_qn, od_c_kn, od_d_qa_norm, od_d_kva_norm, od_d_wq_up, od_d_wkv_up,
              od_d_qn, od_d_kn, router_w, router_b, moe_w_gu, moe_b_gu, moe_w_down, moe_b_down):
    rows = x.shape[1] // GRID_W
    cos, sin = axial_rope_tables(rows, HEAD_DIM)
    cos_r, sin_r = axial_rope_tables(rows, D_ROPE)
    xc = ctx
    for l in range(DEPTH):
        need_ctx = l < DEPTH - 1
        m_lat = jnp.split(adaln(c, adaln_down[l], adaln_up[l], adaln_b[l])[:, None, :], N_MOD, axis=-1)
        m_ctx = jnp.split(adaln(c_ctx, adaln_down[l], adaln_up[l], adaln_b[l])[None, None, :], N_MOD, axis=-1)
        hl = modulate(x, norm_mix[l], m_lat[0], m_lat[1])
        hc = modulate(xc, norm_mix[l], m_ctx[0], m_ctx[1])
        if l % 2 == 0:
            j = l // 2
            lam_init = 0.8 - 0.6 * math.exp(-0.3 * l)
            y_c, y_l = mixer_window_diff(hc, hl, cos, sin, ev_w_in[j], ev_w_out[j], ev_a_qn[j], ev_a_kn[j],
                                         ev_a_sink[j], ev_b_qn[j], ev_b_kn[j], ev_b_lam[j], ev_b_subln[j],
                                         lam_init, need_ctx)
        else:
            j = l // 2
            y_c, y_l = mixer_axial_mla(hc, hl, cos, sin, cos_r, sin_r, od_w_in[j], od_w_out[j], od_c_qn[j],
                                       od_c_kn[j], od_d_qa_norm[j], od_d_kva_norm[j], od_d_wq_up[j],
                                       od_d_wkv_up[j], od_d_qn[j], od_d_kn[j], need_ctx)
        x = x + m_lat[2] * y_l
        hl = modulate(x, norm_ffn[l], m_lat[3], m_lat[4])
        if need_ctx:
            xc = xc + m_ctx[2] * y_c
            hc = modulate(xc, norm_ffn[l], m_ctx[3], m_ctx[4])
            n_ctx = hc.shape[1]
            y = moe_ffn(jnp.concatenate([hc, hl], axis=1), router_w[l], router_b[l], moe_w_gu[l], moe_b_gu[l],
                        moe_w_down[l], moe_b_down[l])
            xc = xc + m_ctx[5] * y[:, :n_ctx]
            x = x + m_lat[5] * y[:, n_ctx:]
        else:
            x = x + m_lat[5] * moe_ffn(hl, router_w[l], router_b[l], moe_w_gu[l], moe_b_gu[l],
                                       moe_w_down[l], moe_b_down[l])
    return x
```

```python
import math
import numpy as np
import concourse.bass as bass
import concourse.mybir as mybir
from concourse.bass_utils import run_bass_kernel_spmd

F32 = mybir.dt.float32
BF16 = mybir.dt.bfloat16
I32 = mybir.dt.int32
AF = mybir.ActivationFunctionType
ALU = mybir.AluOpType
AX = mybir.AxisListType

ENGS = ("pe", "dve", "act", "pool", "sp")
NDMA_SLOTS = 8


class Op:
    __slots__ = ("eng", "emit", "deps", "is_dma", "slot", "slot_val", "slot_prev", "inc", "val", "queue")


class Phase:
    def __init__(self, nc, name="ph"):
        self.nc = nc
        self.name = name
        self.ops = []
        self.last_w = {}
        self.readers = {}
        self.dma_count = {q: 0 for q in ("sp", "pool", "act")}
        self.dma_slot_last = {}
        self._ctxs = []
        self._n = 0

    def sbuf(self, name, shape, dt):
        cm = self.nc.sbuf_tensor(f"{self.name}_{name}", list(shape), dt)
        t = cm.__enter__()
        self._ctxs.append(cm)
        return t

    def psum(self, name, shape, dt=F32):
        cm = self.nc.psum_tensor(f"{self.name}_{name}", list(shape), dt)
        t = cm.__enter__()
        self._ctxs.append(cm)
        return t

    def _add(self, eng, emit, reads, writes, is_dma=False, queue=None):
        op = Op()
        op.eng = eng
        op.emit = emit
        op.is_dma = is_dma
        op.queue = queue
        op.inc = False
        op.val = None
        op.slot = None
        deps = []
        for t in reads:
            w = self.last_w.get(t)
            if w is not None:
                deps.append(w)
        for t in writes:
            w = self.last_w.get(t)
            if w is not None:
                deps.append(w)
            deps.extend(self.readers.get(t, ()))
        seen = set()
        dd = []
        for d in deps:
            if id(d) in seen:
                continue
            seen.add(id(d))
            if (not d.is_dma) and (not is_dma) and d.eng == "pe" and eng == "pe":
                continue
            dd.append(d)
        op.deps = dd
        if is_dma:
            n = self.dma_count[queue]
            self.dma_count[queue] = n + 1
            op.slot = n % NDMA_SLOTS
            op.slot_val = 16 * (n // NDMA_SLOTS + 1)
            op.slot_prev = self.dma_slot_last.get((queue, op.slot))
            self.dma_slot_last[(queue, op.slot)] = op
        for t in reads:
            self.readers.setdefault(t, []).append(op)
        for t in writes:
            self.last_w[t] = op
            self.readers[t] = []
        self.ops.append(op)
        return op

    def pe(self, emit, reads=(), writes=()):
        return self._add("pe", emit, reads, writes)

    def dve(self, emit, reads=(), writes=()):
        return self._add("dve", emit, reads, writes)

    def act(self, emit, reads=(), writes=()):
        return self._add("act", emit, reads, writes)

    def pool(self, emit, reads=(), writes=()):
        return self._add("pool", emit, reads, writes)

    def on(self, eng, emit, reads=(), writes=()):
        return self._add(eng, emit, reads, writes)

    def dma(self, out, in_, reads=(), writes=(), queue="sp", **kw):
        return self._add(queue, lambda e: e.dma_start(out=out, in_=in_, **kw), reads, writes,
                         is_dma=True, queue=queue)

    def selfcheck(self):
        ops = self.ops
        per_eng = {e: [o for o in ops if o.eng == e] for e in ENGS}
        pos = {e: 0 for e in ENGS}
        esem = {e: 0 for e in ENGS}
        dsem = {}
        progress = True
        while progress:
            progress = False
            for e in ENGS:
                while pos[e] < len(per_eng[e]):
                    op = per_eng[e][pos[e]]
                    ok = True
                    for d in op.deps:
                        if d.is_dma:
                            if dsem.get((d.queue, d.slot), 0) < d.slot_val:
                                ok = False
                        elif esem[d.eng] < d.val:
                            ok = False
                    if op.is_dma and op.slot_prev is not None:
                        p = op.slot_prev
                        if dsem.get((p.queue, p.slot), 0) < p.slot_val:
                            ok = False
                    if not ok:
                        break
                    if op.is_dma:
                        dsem[(op.queue, op.slot)] = dsem.get((op.queue, op.slot), 0) + 16
                        assert dsem[(op.queue, op.slot)] == op.slot_val
                    elif op.inc:
                        esem[op.eng] += 1
                        assert esem[op.eng] == op.val
                    pos[e] += 1
                    progress = True
        stuck = {e: (pos[e], len(per_eng[e])) for e in ENGS if pos[e] < len(per_eng[e])}
        assert not stuck, f"phase {self.name}: deadlock {stuck}"

    def finish(self):
        nc = self.nc
        ops = self.ops
        for op in ops:
            for d in op.deps:
                d.inc = True
        cnt = {e: 0 for e in ENGS}
        for op in ops:
            if not op.is_dma and op.inc:
                cnt[op.eng] += 1
                op.val = cnt[op.eng]
        assert max(cnt.values()) < 60000, cnt
        self.selfcheck()
        assert all(16 * (n // NDMA_SLOTS + 1) < 60000 for n in self.dma_count.values()), self.dma_count
        with nc.cleanup_on_exit():
            esem = {e: nc.alloc_semaphore(name=f"{self.name}_c_{e}") for e in ENGS}
            dsem = {}
            for q, n in self.dma_count.items():
                for s in range(min(n, NDMA_SLOTS)):
                    dsem[(q, s)] = nc.alloc_semaphore(name=f"{self.name}_d_{q}{s}")
            per_eng = {e: [o for o in ops if o.eng == e] for e in ENGS}
            slot_last = self.dma_slot_last

            def run(eng_name, e):
                waited = {}

                def wait(key, sem, val):
                    if waited.get(key, 0) >= val:
                        return
                    waited[key] = val
                    e.wait_ge(sem, val)

                for op in per_eng[eng_name]:
                    for d in op.deps:
                        if d.is_dma:
                            wait(("d", d.queue, d.slot), dsem[(d.queue, d.slot)], d.slot_val)
                        else:
                            wait(("e", d.eng), esem[d.eng], d.val)
                    if op.is_dma:
                        p = op.slot_prev
                        if p is not None:
                            wait(("d", p.queue, p.slot), dsem[(p.queue, p.slot)], p.slot_val)
                        op.emit(e).then_inc(dsem[(op.queue, op.slot)], 16)
                    else:
                        ins = op.emit(e)
                        if op.inc:
                            ins.then_inc(esem[op.eng], 1)
                for (q, s), last in slot_last.items():
                    if q == eng_name:
                        wait(("d", q, s), dsem[(q, s)], last.slot_val)

            with nc.Block() as block:
                @block.tensor
                def _(e):
                    run("pe", e)

                @block.vector
                def _(e):
                    run("dve", e)

                @block.scalar
                def _(e):
                    run("act", e)

                @block.gpsimd
                def _(e):
                    run("pool", e)

                @block.sync
                def _(e):
                    run("sp", e)
        for cm in reversed(self._ctxs):
            cm.__exit__(None, None, None)
        self._ctxs = []
        return len(ops)


class Rot:
    def __init__(self, ph, name, n, shape, dt, psum=False):
        self.name = name
        self.n = n
        self.bufs = [(ph.psum if psum else ph.sbuf)(f"{name}{i}", shape, dt) for i in range(n)]
        self.i = 0

    def next(self):
        k = self.i % self.n
        self.i += 1
        return self.bufs[k], (self.name, k)


class Geo:
    def __init__(self, TL=2048, NE=32, DEPTH=4, NCORES=8):
        self.D = 4096
        self.KC = 32
        self.CT = 256
        self.NCT = 2
        self.TL = TL
        self.NTOK = self.CT + TL
        self.NT = self.NTOK // 128
        self.NLT = TL // 128
        self.NE = NE
        self.FF = 384
        self.DEPTH = DEPTH
        self.NC = NCORES
        self.NK = self.CT + NCORES * TL
        self.EV_IN = 7168
        self.OD_IN = 5184
        self.RANK = 512
        self.NMOD = 6


EPS = 1e-6
DBG = {}
THETA = 10000.0
PI = math.pi


def make_identity(ph, name, n, dt):
    idf = ph.sbuf(name + "_f", [128, 128], F32)
    ph.pool(lambda e: e.memset(idf[:], 1.0), writes=[name + "_f"])
    ph.pool(lambda e: e.affine_select(out=idf[:], in_=idf[:], pattern=[[-1, 128]], compare_op=ALU.is_equal,
                                      fill=0.0, base=0, channel_multiplier=1),
            reads=[name + "_f"], writes=[name + "_f"])
    if dt == F32:
        return idf, name + "_f"
    idb = ph.sbuf(name, [128, 128], dt)
    ph.dve(lambda e: e.tensor_copy(idb[:], idf[:]), reads=[name + "_f"], writes=[name])
    return idb, name


def build_pre(nc, g, d):
    ph = Phase(nc, "pre")
    L = g.DEPTH
    idf, idf_t = make_identity(ph, "idf", 128, F32)
    c32 = ph.sbuf("c32", [64, 128], F32)
    ph.dma(c32[:], d["c2"].rearrange("j (k p) -> (j k) p", p=128), writes=["c32"])
    cps = ph.psum("cps", [128, 64], F32)
    ph.pe(lambda e: e.transpose(cps[:], c32[:], idf[0:64, 0:64]), reads=["c32", idf_t], writes=["cps"])
    sT = ph.sbuf("sT", [128, 32, 2], F32)
    ph.act(lambda e: e.activation(out=sT[:].rearrange("p k j -> p j k"), in_=cps[:].rearrange("p (j k) -> p j k", j=2),
                                  func=AF.Silu), reads=["cps"], writes=["sT"])
    down = ph.sbuf("down", [128, 32, 512], F32)
    rps = ph.psum("rps", [2, 512], F32)
    r_sb = ph.sbuf("r_sb", [2, 512], F32)
    rT = ph.sbuf("rT", [128, 4, 2], F32)
    rtp = ph.psum("rtp", [128, 4, 2], F32)
    upr = Rot(ph, "up", 2, [128, 4, 2048], F32)
    br = Rot(ph, "bias", 2, [2, 2048], F32)
    mr = Rot(ph, "modo", 2, [2, 2048], F32)
    mps = Rot(ph, "mps", 2, [2, 512], F32, psum=True)
    for l in range(L):
        ph.dma(down[:], d["adaln_down"][l].rearrange("(k p) n -> p k n", p=128), writes=["down"])
        for k in range(32):
            ph.pe(lambda e, k=k: e.matmul(rps[:], sT[:, k, :], down[:, k, :], start=(k == 0), stop=(k == 31)),
                  reads=["sT", "down"], writes=["rps"])
        ph.dve(lambda e: e.tensor_copy(r_sb[:], rps[:]), reads=["rps"], writes=["r_sb"])
        for kk in range(4):
            ph.pe(lambda e, kk=kk: e.transpose(rtp[:, kk, :], r_sb[:, kk * 128:(kk + 1) * 128], idf[0:2, 0:2]),
                  reads=["r_sb", idf_t], writes=["rtp"])
        ph.dve(lambda e: e.tensor_copy(rT[:], rtp[:]), reads=["rtp"], writes=["rT"])
        for cc in range(12):
            up, upt = upr.next()
            ph.dma(up[:], d["adaln_up"][l][:, cc * 2048:(cc + 1) * 2048].rearrange("(k p) n -> p k n", p=128),
                   writes=[upt])
            bt, btt = br.next()
            ph.dma(bt[:], d["adaln_b"][l:l + 1, cc * 2048:(cc + 1) * 2048].to_broadcast([2, 2048]), writes=[btt])
            mo, mot = mr.next()
            for s in range(4):
                mp, mpt = mps.next()
                for kk in range(4):
                    ph.pe(lambda e, mp=mp, kk=kk, up=up, s=s: e.matmul(mp[:], rT[:, kk, :], up[:, kk, s * 512:(s + 1) * 512],
                                                                     start=(kk == 0), stop=(kk == 3)),
                          reads=["rT", upt], writes=[mpt])
                ph.dve(lambda e, mp=mp, mo=mo, bt=bt, s=s: e.tensor_tensor(out=mo[:, s * 512:(s + 1) * 512], in0=mp[:],
                                                                          in1=bt[:, s * 512:(s + 1) * 512], op=ALU.add),
                       reads=[mpt, btt], writes=[mot])
            ph.dma(d["mod"][l][:, cc * 2048:(cc + 1) * 2048], mo[:], reads=[mot], queue="pool")
    for (dim, key) in ((128, "cs"), (64, "csr")):
        q4 = dim // 4
        half = dim // 2
        it_i = ph.sbuf(f"iti{dim}", [128, dim], I32)
        it_f = ph.sbuf(f"itf{dim}", [128, dim], F32)
        invf = ph.sbuf(f"invf{dim}", [128, dim], F32)
        sgn = ph.sbuf(f"sgn{dim}", [128, dim], F32)
        n1, n2, n3 = f"iti{dim}", f"itf{dim}", f"invf{dim}"
        ph.pool(lambda e, it_i=it_i, q4=q4: e.iota(it_i[:], pattern=[[0, 4], [1, q4]], base=0, channel_multiplier=0),
                writes=[n1])
        ph.dve(lambda e, it_i=it_i, it_f=it_f: e.tensor_copy(it_f[:], it_i[:]), reads=[n1], writes=[n2])
        ph.act(lambda e, it_f=it_f, invf=invf, q4=q4: e.activation(out=invf[:], in_=it_f[:], func=AF.Exp,
                                                                    scale=-math.log(THETA) / q4),
               reads=[n2], writes=[n3])
        ph.pool(lambda e, it_i=it_i, q4=q4: e.iota(it_i[:], pattern=[[0, 2], [1, 2 * q4]], base=0, channel_multiplier=0),
                reads=[n1], writes=[n1])
        ph.dve(lambda e, it_i=it_i, it_f=it_f: e.tensor_copy(it_f[:], it_i[:]), reads=[n1, n3], writes=[n2])
        ph.dve(lambda e, it_f=it_f, sgn=sgn, q4=q4: e.tensor_scalar(sgn[:], it_f[:], float(q4), 2.0, op0=ALU.is_ge, op1=ALU.mult),
               reads=[n2], writes=[f"sgn{dim}"])
        ph.dve(lambda e, sgn=sgn: e.tensor_scalar(sgn[:], sgn[:], -1.0, None, op0=ALU.add),
               reads=[f"sgn{dim}"], writes=[f"sgn{dim}"])
        mpi = ph.sbuf(f"mpi{dim}", [128, 1], F32)
        ph.dve(lambda e, mpi=mpi: e.memset(mpi[:], -PI), writes=[f"mpi{dim}"])
        posr = Rot(ph, f"pos{dim}", 2, [128, 2], F32)
        angr = Rot(ph, f"ang{dim}", 2, [128, dim], F32)
        argr = Rot(ph, f"arg{dim}", 2, [128, 2, dim], F32)
        kfr = Rot(ph, f"kf{dim}", 2, [128, dim], F32)
        kir = Rot(ph, f"ki{dim}", 2, [128, dim], I32)
        outr = Rot(ph, f"cso{dim}", 2, [128, 2, dim], F32)
        for t in range(g.NLT):
            pt, ptt = posr.next()
            ph.dma(pt[:], d["pos"][t * 128:(t + 1) * 128, :], writes=[ptt])
            an, ant = angr.next()
            ph.dve(lambda e, an=an, pt=pt, invf=invf, half=half: e.tensor_scalar(an[:, 0:half], invf[:, 0:half], pt[:, 0:1], None, op0=ALU.mult),
                   reads=[ptt, n3], writes=[ant])
            ph.dve(lambda e, an=an, pt=pt, invf=invf, half=half, dim=dim: e.tensor_scalar(an[:, half:dim], invf[:, half:dim], pt[:, 1:2], None, op0=ALU.mult),
                   reads=[ptt, n3, ant], writes=[ant])
            ar, art = argr.next()
            for j, shift in ((0, 0.5 * PI), (1, 0.0)):
                kf, kft = kfr.next()
                ki, kit = kir.next()
                ph.dve(lambda e, ar=ar, an=an, j=j, shift=shift: e.tensor_scalar(ar[:, j, :], an[:], shift, None, op0=ALU.add),
                       reads=[ant, art], writes=[art])
                ph.dve(lambda e, kf=kf, ar=ar, j=j: e.tensor_scalar(kf[:], ar[:, j, :], 1.0 / (2 * PI), None, op0=ALU.mult),
                       reads=[art], writes=[kft])
                ph.dve(lambda e, kf=kf, ki=ki: e.tensor_copy(ki[:], kf[:]), reads=[kft], writes=[kit])
                ph.dve(lambda e, kf=kf, ki=ki: e.tensor_copy(kf[:], ki[:]), reads=[kit], writes=[kft])
                ph.dve(lambda e, kf=kf, ar=ar, j=j: e.scalar_tensor_tensor(out=ar[:, j, :], in0=kf[:], scalar=-2 * PI, in1=ar[:, j, :],
                                                                         op0=ALU.mult, op1=ALU.add),
                       reads=[kft, art], writes=[art])
                ph.dve(lambda e, kf=kf, ar=ar, j=j: e.tensor_scalar(kf[:], ar[:, j, :], PI, 2 * PI, op0=ALU.is_gt, op1=ALU.mult),
                       reads=[art], writes=[kft])
                ph.dve(lambda e, kf=kf, ar=ar, j=j: e.tensor_tensor(out=ar[:, j, :], in0=ar[:, j, :], in1=kf[:], op=ALU.subtract),
                       reads=[art, kft], writes=[art])
                ph.dve(lambda e, kf=kf, ar=ar, j=j: e.tensor_scalar(kf[:], ar[:, j, :], -PI, 2 * PI, op0=ALU.is_lt, op1=ALU.mult),
                       reads=[art], writes=[kft])
                ph.dve(lambda e, kf=kf, ar=ar, j=j: e.tensor_tensor(out=ar[:, j, :], in0=ar[:, j, :], in1=kf[:], op=ALU.add),
                       reads=[art, kft], writes=[art])
                ph.dve(lambda e, ar=ar, j=j: e.tensor_scalar(ar[:, j, :], ar[:, j, :], -3.141592, 3.141592, op0=ALU.max, op1=ALU.min),
                       reads=[art], writes=[art])
            ot, ott = outr.next()
            ph.act(lambda e, ot=ot, ar=ar, mpi=mpi: e.activation(out=ot[:], in_=ar[:], func=AF.Sin),
                   reads=[art], writes=[ott])
            ph.dve(lambda e, ot=ot, sgn=sgn: e.tensor_tensor(out=ot[:, 1, :], in0=ot[:, 1, :], in1=sgn[:], op=ALU.mult),
                   reads=[ott, f"sgn{dim}"], writes=[ott])
            ph.dma(d[key][t * 128:(t + 1) * 128], ot[:], reads=[ott], queue="pool")
    return ph.finish()


_NP2DT = {np.dtype(np.float32): F32, np.dtype(np.int32): I32}


def _dt_of(a):
    import ml_dtypes
    if a.dtype == np.dtype(ml_dtypes.bfloat16):
        return BF16
    return _NP2DT[a.dtype]


def launch(build_fn, g, in_maps, out_specs, trace=False):
    nc = bass.Bass("TRN2", target_bir_lowering=False)
    d = {}
    for name, a in in_maps[0].items():
        d[name] = nc.dram_tensor(name, list(a.shape), _dt_of(a), kind="ExternalInput").ap()
    for name, (shape, dt) in out_specs.items():
        d[name] = nc.dram_tensor(name, list(shape), dt, kind="ExternalOutput").ap()
    build_fn(nc, g, d)
    res = run_bass_kernel_spmd(nc, in_maps, core_ids=list(range(len(in_maps))), trace=trace)
    return res


class NormT:
    def __init__(self, ph, g, ident, ident_t):
        self.ph = ph
        self.g = g
        self.ident = ident
        self.ident_t = ident_t
        self.xr = Rot(ph, "nx", 1, [128, 4096], F32)
        self.jr = Rot(ph, "njunk", 1, [128, 4096], BF16)
        self.hr = Rot(ph, "nh", 2, [128, 1024], BF16)
        self.sr = Rot(ph, "nss", 2, [128, 1], F32)
        self.tp = Rot(ph, "ntp", 2, [128, 4, 128], BF16, psum=True)
        self.k = 0

    def load_mod(self, gm, sh, gmt, sht, d_mod_l, d_gain_l, j, off_shift, off_scale):
        ph = self.ph
        ph.dma(gm[:], d_mod_l[j:j + 1, off_scale:off_scale + 4096].to_broadcast([128, 4096]), writes=[gmt])
        ph.dma(sh[:], d_gain_l.to_broadcast([128, 4096]), writes=[sht])
        ph.dve(lambda e: e.scalar_tensor_tensor(out=gm[:], in0=gm[:], scalar=1.0, in1=sh[:], op0=ALU.add, op1=ALU.mult),
                reads=[gmt, sht], writes=[gmt])
        ph.dma(sh[:], d_mod_l[j:j + 1, off_shift:off_shift + 4096].to_broadcast([128, 4096]), reads=[], writes=[sht])

    def tile(self, x_rows, gm, sh, gmt, sht, hT, hTt, col0, x_keep=None):
        ph = self.ph
        if x_keep is None:
            xt, xtt = self.xr.next()
            ph.dma(xt[:], x_rows, writes=[xtt])
        else:
            xt, xtt = x_keep
        jk, jkt = self.jr.next()
        ss, sst = self.sr.next()
        ph.act(lambda e: e.activation(out=jk[:], in_=xt[:], func=AF.Square, accum_out=ss[:]), reads=[xtt], writes=[jkt, sst])
        ph.dve(lambda e: e.tensor_scalar(ss[:], ss[:], 1.0 / 4096, EPS, op0=ALU.mult, op1=ALU.add), reads=[sst], writes=[sst])
        ph.act(lambda e: e.activation(out=ss[:], in_=ss[:], func=AF.Sqrt), reads=[sst], writes=[sst])
        ph.dve(lambda e: e.reciprocal(ss[:], ss[:]), reads=[sst], writes=[sst])
        for b in range(4):
            h, ht = self.hr.next()
            cs = slice(b * 1024, (b + 1) * 1024)
            eng = "pool"
            ph.on("dve", lambda e, h=h, cs=cs: e.scalar_tensor_tensor(out=xt[:, cs], in0=xt[:, cs], scalar=ss[:, 0:1], in1=gm[:, cs],
                                                                   op0=ALU.mult, op1=ALU.mult),
                  reads=[xtt, sst, gmt], writes=[xtt])
            ph.on(eng, lambda e, h=h, cs=cs: e.tensor_tensor(out=h[:], in0=xt[:, cs], in1=sh[:, cs], op=ALU.add),
                  reads=[xtt, sht], writes=[ht])
            for q in range(2):
                tp, tpt = self.tp.next()
                for i in range(4):
                    ph.pe(lambda e, tp=tp, h=h, i=i, q=q: e.transpose(tp[:, i, :], h[:, (q * 4 + i) * 128:(q * 4 + i + 1) * 128], self.ident[:]),
                          reads=[ht, self.ident_t], writes=[tpt])
                kc0 = b * 8 + q * 4
                self.k += 1
                if self.k % 2 == 0:
                    ph.act(lambda e, tp=tp, kc0=kc0: e.activation(out=hT[:, kc0:kc0 + 4, col0:col0 + 128], in_=tp[:], func=AF.Copy),
                           reads=[tpt], writes=[hTt])
                else:
                    ph.dve(lambda e, tp=tp, kc0=kc0: e.tensor_copy(hT[:, kc0:kc0 + 4, col0:col0 + 128], tp[:]),
                           reads=[tpt], writes=[hTt])


def load_w_chunk(ph, wt, wtt, w_dram, c0, ncols, kchunks=32, queue="pool"):
    for k0 in range(0, kchunks, 8):
        k1 = min(k0 + 8, kchunks)
        ph.dma(wt[:, k0:k1, 0:ncols],
               w_dram[k0 * 128:k1 * 128, c0:c0 + ncols].rearrange("(k p) n -> p k n", p=128),
               writes=[wtt], queue=queue)


def tile_groups(g, gt=4):
    groups = [list(range(0, g.NCT))]
    t = g.NCT
    while t < g.NT:
        groups.append(list(range(t, min(t + gt, g.NT))))
        t += gt
    return groups


def build_proj(nc, g, d, l):
    even = (l % 2 == 0)
    ph = Phase(nc, f"pj{l}")
    NTOK = g.NTOK
    ident, ident_t = make_identity(ph, "ident", 128, BF16)
    nt = NormT(ph, g, ident, ident_t)
    gm = ph.sbuf("gm", [128, 4096], F32)
    sh = ph.sbuf("sh", [128, 4096], F32)
    hT = ph.sbuf("hT", [128, 32, 512], BF16)
    wr = Rot(ph, "w", 2, [128, 32, 512], BF16)
    IN = g.EV_IN if even else g.OD_IN
    gains = {}

    def gain_tile(name, ap_row, n):
        t = ph.sbuf("g_" + name, [128, n], F32)
        ph.dma(t[:], ap_row.to_broadcast([128, n]), writes=["g_" + name])
        gains[name] = (t, "g_" + name)

    if even:
        for nm in ("a_qn", "a_kn", "b_qn", "b_kn"):
            gain_tile(nm, d[nm], 128)
        plan = []
        for c in range(4):
            plan.append(("qk", "a_qn", "QT", 4 * c))
        for c in range(4):
            plan.append(("qk", "b_qn", "QT", 16 + 4 * c))
        plan.append(("qk", "a_kn", "KT", 0))
        plan.append(("v", None, "V", 0))
        plan.append(("qk", "b_kn", "KT", 4))
        plan.append(("qk", "b_kn", "KT", 8))
        plan.append(("v", None, "V", 512))
        plan.append(("v", None, "V", 1024))
    else:
        for nm in ("c_qn", "c_kn"):
            gain_tile(nm, d[nm], 128)
        gain_tile("qa", d["d_qa_norm"], 1536)
        gain_tile("kva", d["d_kva_norm"], 512)
        plan = []
        for c in range(4):
            plan.append(("qk", "c_qn", "QT", 4 * c))
        for c in range(3):
            plan.append(("lat", "qa", "qaT", 4 * c))
        plan.append(("qk", "c_kn", "KT", 0))
        plan.append(("v", None, "V", 0))
        plan.append(("lat", "kva", "kvaT", 0))
        plan.append(("kr", None, "kr", 0))
    pps = Rot(ph, "pps", 3, [128, 512], F32, psum=True)
    tps = Rot(ph, "tps", 2, [128, 4, 128], BF16, psum=True)
    p_r = Rot(ph, "p", 2, [128, 512], F32)
    sq_r = Rot(ph, "sq", 1, [128, 512], F32)
    ss_r = Rot(ph, "ss", 2, [128, 4], F32)
    y_r = Rot(ph, "y", 2, [128, 512], F32)
    t1_r = Rot(ph, "t1", 2, [128, 512], F32)
    t2_r = Rot(ph, "t2", 2, [128, 512], F32)
    yb_r = Rot(ph, "yb", 2, [128, 512], BF16)
    st_r = Rot(ph, "st", 2, [128, 4, 128], BF16)
    cs_r = Rot(ph, "cs", 2, [128, 2, 128], F32)
    ssq_t = {}
    if not even:
        ssq_q = ph.sbuf("ssq_q", [128, g.NT], F32)
        ssq_kv = ph.sbuf("ssq_kv", [128, g.NT], F32)
        ssq_tmp = Rot(ph, "ssq_tmp", 2, [128, 1], F32)
        ph.dve(lambda e: e.memset(ssq_q[:], 0.0), writes=["ssq_q"])
        ph.dve(lambda e: e.memset(ssq_kv[:], 0.0), writes=["ssq_kv"])
    evk = [0]
    for grp in tile_groups(g):
        j = 1 if grp[0] < g.NCT else 0
        nt.load_mod(gm, sh, "gm", "sh", d["mod"][l], d["norm_mix"][l:l + 1, :], j, 0, 4096)
        for ti, t in enumerate(grp):
            nt.tile(d["x"][t * 128:(t + 1) * 128, :], gm, sh, "gm", "sh", hT, "hT", ti * 128)
        cs_tiles = {}
        for ci, (kind, gname, dest, i0) in enumerate(plan):
            c0 = ci * 512
            ncols = min(512, IN - c0)
            w, wt_ = wr.next()
            load_w_chunk(ph, w, wt_, d["w_in"], c0, ncols)
            for ti, t in enumerate(grp):
                pp, ppt = pps.next()
                for k in range(32):
                    ph.pe(lambda e, pp=pp, k=k, w=w, ti=ti, ncols=ncols: e.matmul(pp[:, 0:ncols], hT[:, k, ti * 128:(ti + 1) * 128], w[:, k, 0:ncols],
                                                                                 start=(k == 0), stop=(k == 31)),
                          reads=["hT", wt_], writes=[ppt])
                rows = slice(t * 128, (t + 1) * 128)
                is_lat = t >= g.NCT
                if kind == "v":
                    yb, ybt = yb_r.next()
                    ph.act(lambda e, yb=yb, pp=pp: e.activation(out=yb[:], in_=pp[:], func=AF.Copy), reads=[ppt], writes=[ybt])
                    ph.dma(d["V"][rows, i0:i0 + 512], yb[:], reads=[ybt], queue="sp")
                    continue
                if kind == "kr":
                    p, pt = p_r.next()
                    ph.act(lambda e, p=p, pp=pp: e.activation(out=p[:, 0:64], in_=pp[:, 0:64], func=AF.Copy), reads=[ppt], writes=[pt])
                    ph.dma(d["kr"][rows, :], p[:, 0:64], reads=[pt], queue="sp")
                    continue
                p, pt = p_r.next()
                ph.act(lambda e, p=p, pp=pp: e.activation(out=p[:], in_=pp[:], func=AF.Copy), reads=[ppt], writes=[pt])
                sq, sqt = sq_r.next()
                ph.dve(lambda e, sq=sq, p=p: e.tensor_tensor(out=sq[:], in0=p[:], in1=p[:], op=ALU.mult), reads=[pt], writes=[sqt])
                gt_, gtt = gains[gname]
                yb, ybt = yb_r.next()
                if kind == "lat":
                    tmp, tmpt = ssq_tmp.next()
                    acc, acct = (ssq_q, "ssq_q") if dest == "qaT" else (ssq_kv, "ssq_kv")
                    ph.dve(lambda e, tmp=tmp, sq=sq: e.tensor_reduce(out=tmp[:], in_=sq[:], axis=AX.X, op=ALU.add), reads=[sqt], writes=[tmpt])
                    ph.dve(lambda e, tmp=tmp, acc=acc, t=t: e.tensor_tensor(out=acc[:, t:t + 1], in0=acc[:, t:t + 1], in1=tmp[:], op=ALU.add),
                           reads=[tmpt, acct], writes=[acct])
                    gc0 = i0 * 128
                    ph.pool(lambda e, yb=yb, p=p, gt_=gt_, gc0=gc0: e.tensor_tensor(out=yb[:], in0=p[:], in1=gt_[:, gc0:gc0 + 512], op=ALU.mult),
                            reads=[pt, gtt], writes=[ybt])
                else:
                    ss, sst = ss_r.next()
                    ph.dve(lambda e, ss=ss, sq=sq: e.tensor_reduce(out=ss[:], in_=sq[:].rearrange("p (h d) -> p h d", d=128), axis=AX.X, op=ALU.add),
                           reads=[sqt], writes=[sst])
                    ph.dve(lambda e, ss=ss: e.tensor_scalar(ss[:], ss[:], 1.0 / 128, EPS, op0=ALU.mult, op1=ALU.add), reads=[sst], writes=[sst])
                    ph.act(lambda e, ss=ss: e.activation(out=ss[:], in_=ss[:], func=AF.Sqrt), reads=[sst], writes=[sst])
                    ph.dve(lambda e, ss=ss: e.reciprocal(ss[:], ss[:]), reads=[sst], writes=[sst])
                    y, yt = y_r.next()
                    ph.dve(lambda e, y=y, p=p, ss=ss: e.tensor_tensor(out=y[:].rearrange("p (h d) -> p h d", d=128),
                                                                    in0=p[:].rearrange("p (h d) -> p h d", d=128),
                                                                    in1=ss[:].unsqueeze(2).to_broadcast([128, 4, 128]), op=ALU.mult),
                           reads=[pt, sst], writes=[yt])
                    if not is_lat:
                        ph.pool(lambda e, y=y, yb=yb, gt_=gt_: e.tensor_tensor(out=yb[:].rearrange("p (h d) -> p h d", d=128),
                                                                              in0=y[:].rearrange("p (h d) -> p h d", d=128),
                                                                              in1=gt_[:].unsqueeze(1).to_broadcast([128, 4, 128]), op=ALU.mult),
                                reads=[yt, gtt], writes=[ybt])
                    else:
                        ph.pool(lambda e, y=y, gt_=gt_: e.tensor_tensor(out=y[:].rearrange("p (h d) -> p h d", d=128),
                                                                       in0=y[:].rearrange("p (h d) -> p h d", d=128),
                                                                       in1=gt_[:].unsqueeze(1).to_broadcast([128, 4, 128]), op=ALU.mult),
                                reads=[yt, gtt], writes=[yt])
                        if t not in cs_tiles:
                            cst, cstt = cs_r.next()
                            lt = t - g.NCT
                            ph.dma(cst[:], d["cs"][lt * 128:(lt + 1) * 128], writes=[cstt])
                            cs_tiles = {t: (cst, cstt)}
                        cst, cstt = cs_tiles[t]
                        t1, t1t = t1_r.next()
                        t2, t2t = t2_r.next()
                        ph.pool(lambda e, t1=t1, y=y, cst=cst: e.tensor_tensor(out=t1[:].rearrange("p (h d) -> p h d", d=128),
                                                                              in0=y[:].rearrange("p (h d) -> p h d", d=128),
                                                                              in1=cst[:, 0:1, :].to_broadcast([128, 4, 128]), op=ALU.mult),
                                reads=[yt, cstt], writes=[t1t])
                        yv = y[:].rearrange("p (h a s f) -> p (h a) s f", a=2, s=2, f=32)
                        t2v = t2[:].rearrange("p (h a s f) -> p (h a) s f", a=2, s=2, f=32)
                        snv = cst[:, 1, :].rearrange("p (a s f) -> p a s f", a=2, s=2)
                        for s_ in range(2):
                            ph.dve(lambda e, t2v=t2v, yv=yv, snv=snv, s_=s_: e.tensor_tensor(
                                out=t2v[:, :, s_, :].rearrange("p (h a) f -> p h a f", a=2),
                                in0=yv[:, :, 1 - s_, :].rearrange("p (h a) f -> p h a f", a=2),
                                in1=snv[:, :, s_, :].unsqueeze(1).to_broadcast([128, 4, 2, 32]), op=ALU.mult),
                                reads=[yt, cstt, t2t], writes=[t2t])
                        ph.pool(lambda e, yb=yb, t1=t1, t2=t2: e.tensor_tensor(out=yb[:], in0=t1[:], in1=t2[:], op=ALU.add),
                                reads=[t1t, t2t], writes=[ybt])
                tp, tpt = tps.next()
                for i in range(4):
                    ph.pe(lambda e, tp=tp, yb=yb, i=i: e.transpose(tp[:, i, :], yb[:, i * 128:(i + 1) * 128], ident[:]),
                          reads=[ybt, ident_t], writes=[tpt])
                st, stt = st_r.next()
                evk[0] += 1
                if evk[0] % 2 == 0:
                    ph.act(lambda e, st=st, tp=tp: e.activation(out=st[:], in_=tp[:], func=AF.Copy), reads=[tpt], writes=[stt])
                else:
                    ph.dve(lambda e, st=st, tp=tp: e.tensor_copy(st[:], tp[:]), reads=[tpt], writes=[stt])
                ph.dma(d[dest][i0:i0 + 4, :, t * 128:(t + 1) * 128].rearrange("v p t -> p v t"), st[:], reads=[stt], queue="sp")
    if not even:
        for t in range(g.NT):
            ph.dma(d["ssq_q"][t * 128:(t + 1) * 128, :], ssq_q[:, t:t + 1], reads=["ssq_q"], queue="sp")
            ph.dma(d["ssq_kv"][t * 128:(t + 1) * 128, :], ssq_kv[:, t:t + 1], reads=["ssq_kv"], queue="sp")
    return ph.finish()


class AttRes:
    def __init__(self, ph, nq, ndv):
        self.ph = ph
        self.nq = nq
        self.S = Rot(ph, "S", 2, [128, 512], F32, psum=True)
        self.P = Rot(ph, "P", 4, [128, 512], BF16)
        self.acc = [[ph.psum(f"acc{s}_{i}", [128, 512], F32) for i in range(3)] for s in range(2)]
        self.acc_i = 0
        self.ones = ph.sbuf("ones_b", [128, 128], BF16)
        ph.dve(lambda e: e.memset(self.ones[:], 1.0), writes=["ones_b"])
        self.rc = Rot(ph, "rc", 2, [128, 512], F32)

    def next_acc(self):
        s = self.acc_i % 2
        self.acc_i += 1
        return self.acc[s], [("acc", s, i) for i in range(3)]


def attend(ph, A, q_list, nq, key_tiles, ndv, scale, q_shape=None):
    acc, acct = A.next_acc()
    nkt = len(key_tiles)
    for j, (k_list, v_ap, vtok, mask) in enumerate(key_tiles):
        S, St = A.S.next()
        if q_shape is None:
            for ci, ((kap, ktok), (qap, qtok)) in enumerate(zip(k_list, q_list)):
                ph.pe(lambda e, S=S, kap=kap, qap=qap, ci=ci: e.matmul(S[:, 0:nq], kap, qap, start=(ci == 0), stop=(ci == len(q_list) - 1)),
                      reads=[ktok, qtok], writes=[St])
        else:
            (kap, ktok), (qfn, qtok) = k_list[0], q_list[0]
            for hq in range(q_shape[0]):
                ph.pe(lambda e, S=S, kap=kap, qfn=qfn, hq=hq: e.matmul(S[:, hq * 128:(hq + 1) * 128], kap, qfn(hq), start=True, stop=True),
                      reads=[ktok, qtok], writes=[St])
        P, Pt = A.P.next()
        ph.act(lambda e, P=P, S=S: e.activation(out=P[:, 0:nq], in_=S[:, 0:nq], func=AF.Exp, scale=scale), reads=[St], writes=[Pt])
        if mask is not None:
            map_, mtok = mask
            ph.dve(lambda e, P=P, map_=map_: e.tensor_tensor(out=P[:, 0:nq], in0=P[:, 0:nq], in1=map_, op=ALU.mult),
                   reads=[Pt, mtok], writes=[Pt])
        for dc in range(ndv):
            ph.pe(lambda e, dc=dc, P=P, v_ap=v_ap, j=j: e.matmul(acc[dc][:, 0:nq], v_ap[:, dc * 128:(dc + 1) * 128], P[:, 0:nq],
                                                               start=(j == 0), stop=(j == nkt - 1)),
                  reads=[Pt, vtok], writes=[acct[dc]])
        ph.pe(lambda e, P=P, j=j: e.matmul(acc[2][:, 0:nq], A.ones[:], P[:, 0:nq], start=(j == 0), stop=(j == nkt - 1)),
              reads=[Pt, "ones_b"], writes=[acct[2]])
    return acc, acct


def load_kT(ph, dst, dtok, src, ncols, parts=128):
    step = 4096
    for c0 in range(0, ncols, step):
        c1 = min(ncols, c0 + step)
        ph.dma(dst[0:parts, c0:c1], src[:, c0:c1], writes=[dtok])


def load_v(ph, dst, dtok, src_rows, ntiles, c0, ncols):
    step = 16
    for t0 in range(0, ntiles, step):
        t1 = min(ntiles, t0 + step)
        ph.dma(dst[:, t0:t1, :], src_rows[t0 * 128:t1 * 128, c0:c0 + ncols].rearrange("(t p) c -> p t c", p=128), writes=[dtok])


def q_blocks(g):
    out = [(0, g.CT, True)]
    nq = min(512, g.TL)
    for b in range(g.TL // nq):
        out.append((g.CT + b * nq, nq, False))
    return out


def finish_simple(ph, A, acc, acct, nq, ndv, yt_dst, ystage, extra_sum=None):
    rc, rct = A.rc.next()
    if extra_sum is not None:
        ea, et = extra_sum
        ph.dve(lambda e: e.tensor_scalar(rc[:, 0:nq], acc[2][:, 0:nq], ea, None, op0=ALU.add), reads=[acct[2], et], writes=[rct])
        ph.dve(lambda e: e.reciprocal(rc[:, 0:nq], rc[:, 0:nq]), reads=[rct], writes=[rct])
    else:
        ph.dve(lambda e: e.reciprocal(rc[:, 0:nq], acc[2][:, 0:nq]), reads=[acct[2]], writes=[rct])
    for dc in range(ndv):
        ys, yst = ystage.next()
        ph.dve(lambda e, dc=dc, ys=ys: e.tensor_tensor(out=ys[:, 0:nq], in0=acc[dc][:, 0:nq], in1=rc[:, 0:nq], op=ALU.mult),
               reads=[acct[dc], rct], writes=[yst])
        ph.dma(yt_dst[dc], ys[:, 0:nq], reads=[yst], queue="sp")


def build_att_a(nc, g, d, l):
    ph = Phase(nc, f"aa{l}")
    NTOK, TL, CT = g.NTOK, g.TL, g.CT
    A = AttRes(ph, 512, 1)
    scale = 128 ** -0.5
    qT = ph.sbuf("qT", [128, 16, NTOK], BF16)
    for h0 in range(0, 16, 4):
        ph.dma(qT[:, h0:h0 + 4, :], d["QT"][h0:h0 + 4].rearrange("v p t -> p v t"), writes=["qT"])
    kw = ph.sbuf("kw", [128, 4, TL + 256], BF16)
    ph.dma(kw[:], d["KAw"].rearrange("v p t -> p v t"), writes=["kw"])
    kc = ph.sbuf("kc", [128, 4, CT], BF16)
    ph.dma(kc[:], d["KT"][0:4, :, 0:CT].rearrange("v p t -> p v t"), writes=["kc"])
    nwt = (TL + 256) // 128
    vw = ph.sbuf("vw", [128, nwt, 512], BF16)
    load_v(ph, vw, "vw", d["VAw"], nwt, 0, 512)
    vc = ph.sbuf("vc", [128, 2, 512], BF16)
    load_v(ph, vc, "vc", d["V"], 2, 0, 512)
    mf = ph.sbuf("mf", [128, 512], F32)
    mf1 = ph.sbuf("mf1", [128, 128], F32)
    flags = ph.sbuf("flags", [128, 2], F32)
    ph.dma(flags[:], d["flags"], writes=["flags"])
    masks = {}
    idm = ph.sbuf("idm", [128, 128], F32)
    ph.pool(lambda e: e.memset(idm[:], 1.0), writes=["idm"])
    ph.pool(lambda e: e.affine_select(out=idm[:], in_=idm[:], pattern=[[-1, 128]], compare_op=ALU.is_equal,
                                      fill=0.0, base=0, channel_multiplier=1), reads=["idm"], writes=["idm"])
    for nm in ("prev", "next"):
        if nm == "prev":
            ph.pool(lambda e: e.memset(mf1[:], 1.0), reads=["mf"], writes=["mf1"])
            ph.pool(lambda e: e.affine_select(out=mf1[:], in_=mf1[:], pattern=[[-1, 128]], compare_op=ALU.is_ge,
                                              fill=0.0, base=0, channel_multiplier=1), reads=["mf1"], writes=["mf1"])
        else:
            ph.dve(lambda e: e.tensor_tensor(out=mf1[:], in0=idm[:], in1=mf1[:], op=ALU.subtract), reads=["idm", "mf1", "mf"], writes=["mf1"])
            ph.dve(lambda e: e.tensor_scalar(mf1[:], mf1[:], 1.0, None, op0=ALU.add), reads=["mf1"], writes=["mf1"])
        ph.dve(lambda e: e.tensor_copy(mf[:].rearrange("p (h q) -> p h q", h=4), mf1[:].unsqueeze(1).to_broadcast([128, 4, 128])),
               reads=["mf1"], writes=["mf"])
        mb = ph.sbuf("m_" + nm, [128, 512], BF16)
        ph.dve(lambda e, mb=mb: e.tensor_copy(mb[:], mf[:]), reads=["mf"], writes=["m_" + nm])
        masks[nm] = (mb, "m_" + nm)
        me = ph.sbuf("me_" + nm, [128, 512], BF16)
        col = 0 if nm == "prev" else 1
        ph.dve(lambda e, me=me, col=col: e.tensor_scalar(me[:], mf[:], flags[:, col:col + 1], None, op0=ALU.mult),
               reads=["mf", "flags"], writes=["me_" + nm])
        masks[nm + "_e"] = (me, "me_" + nm)
    esink = ph.sbuf("esink", [128, 16], F32)
    ph.dma(esink[:], d["a_sink"].to_broadcast([128, 16]), writes=["esink"])
    ph.act(lambda e: e.activation(out=esink[:], in_=esink[:], func=AF.Exp), reads=["esink"], writes=["esink"])
    ystage = Rot(ph, "ys", 3, [128, 512], BF16)
    for t in range(g.NT if not DBG.get("a_setup_only") else 0):
        is_ctx = t < g.NCT
        for kvh in range(4):
            q_list = [((lambda hq, kvh=kvh, t=t: qT[:, 4 * kvh + hq, t * 128:(t + 1) * 128]), "qT")]
            kts = []
            if not is_ctx:
                lt = t - g.NCT
                for off, nm in ((0, "prev"), (1, None), (2, "next")):
                    wt = lt + off
                    m = None
                    if nm == "prev":
                        m = masks["prev_e"] if lt == 0 else masks["prev"]
                    elif nm == "next":
                        m = masks["next_e"] if lt == g.NLT - 1 else masks["next"]
                    kts.append(([(kw[:, kvh, wt * 128:(wt + 1) * 128], "kw")], vw[:, wt, kvh * 128:(kvh + 1) * 128], "vw",
                                None if (m is None or DBG.get("a_nomask")) else (m[0][:], m[1])))
            for ct in range(2):
                kts.append(([(kc[:, kvh, ct * 128:(ct + 1) * 128], "kc")], vc[:, ct, kvh * 128:(kvh + 1) * 128], "vc", None))
            acc, acct = attend(ph, A, q_list, 512, kts, 1, scale, q_shape=(4, 128))
            rc, rct = A.rc.next()
            for gq in range(4):
                h = 4 * kvh + gq
                ph.dve(lambda e, gq=gq, h=h, rc=rc, acc=acc: e.tensor_scalar(rc[:, gq * 128:(gq + 1) * 128], acc[2][:, gq * 128:(gq + 1) * 128],
                                                                            esink[:, h:h + 1], None, op0=ALU.add),
                       reads=[acct[2], "esink"], writes=[rct])
            ph.dve(lambda e, rc=rc: e.reciprocal(rc[:], rc[:]), reads=[rct], writes=[rct])
            ys, yst = ystage.next()
            ph.dve(lambda e, ys=ys, acc=acc, rc=rc: e.tensor_tensor(out=ys[:], in0=acc[0][:], in1=rc[:], op=ALU.mult), reads=[acct[0], rct], writes=[yst])
            ph.dma(d["YT"][4 * kvh:4 * kvh + 4, :, t * 128:(t + 1) * 128].rearrange("v p t -> p v t"),
                   ys[:].rearrange("p (h q) -> p h q", h=4), reads=[yst], queue="sp")
    return ph.finish()


def key_tile_list(g, kaps, vap, vtok, is_ctx, ndvcols):
    nkt = g.NCT if is_ctx else g.NK // 128
    out = []
    for j in range(nkt):
        k_list = [(ka[0:parts, j * 128:(j + 1) * 128], kt) for (ka, kt, parts) in kaps]
        out.append((k_list, vap[:, j, 0:ndvcols], vtok, None))
    return out


def build_att_b(nc, g, d, l):
    ph = Phase(nc, f"ab{l}")
    NTOK, NK = g.NTOK, g.NK
    nkt = NK // 128
    lam_init = 0.8 - 0.6 * math.exp(-0.3 * l)
    A = AttRes(ph, 512, 2)
    scale = 128 ** -0.5
    bl = ph.sbuf("bl", [1, 512], F32)
    ph.dma(bl[:], d["b_lam"], writes=["bl"])
    pr = ph.sbuf("pr", [1, 256], F32)
    ph.dve(lambda e: e.tensor_tensor(out=pr[:].rearrange("p (a d) -> p a d", a=2), in0=bl[:].rearrange("p (a b d) -> p a b d", a=2, b=2)[:, :, 0, :],
                                     in1=bl[:].rearrange("p (a b d) -> p a b d", a=2, b=2)[:, :, 1, :], op=ALU.mult), reads=["bl"], writes=["pr"])
    l2 = ph.sbuf("l2", [1, 2], F32)
    ph.dve(lambda e: e.tensor_reduce(out=l2[:], in_=pr[:].rearrange("p (a d) -> p a d", a=2), axis=AX.X, op=ALU.add), reads=["pr"], writes=["l2"])
    ph.act(lambda e: e.activation(out=l2[:], in_=l2[:], func=AF.Exp), reads=["l2"], writes=["l2"])
    lam1 = ph.sbuf("lam1", [1, 1], F32)
    ph.dve(lambda e: e.tensor_tensor(out=lam1[:], in0=l2[:, 0:1], in1=l2[:, 1:2], op=ALU.subtract), reads=["l2"], writes=["lam1"])
    ph.dve(lambda e: e.tensor_scalar(lam1[:], lam1[:], -1.0, -lam_init, op0=ALU.mult, op1=ALU.add), reads=["lam1"], writes=["lam1"])
    ones1 = ph.sbuf("ones1", [1, 128], F32)
    ph.dve(lambda e: e.memset(ones1[:], 1.0), writes=["ones1"])
    onesf = ph.sbuf("onesf", [128, 128], F32)
    ph.dve(lambda e: e.memset(onesf[:], 1.0), writes=["onesf"])
    lps, lpst = A.S.next()
    ph.pe(lambda e: e.matmul(lps[:, 0:1], ones1[:], lam1[:], start=True, stop=True), reads=["ones1", "lam1"], writes=[lpst])
    nlam = ph.sbuf("nlam", [128, 1], F32)
    ph.dve(lambda e: e.tensor_copy(nlam[:], lps[:, 0:1]), reads=[lpst], writes=["nlam"])
    gsub = ph.sbuf("gsub", [128, 2], F32)
    ph.dma(gsub[:], d["b_subln"].rearrange("o (c p) -> p (o c)", p=128), writes=["gsub"], allow_slow_non_contiguous=True)
    ph.dve(lambda e: e.tensor_scalar(gsub[:], gsub[:], 1.0 - lam_init, None, op0=ALU.mult), reads=["gsub"], writes=["gsub"])
    qT = ph.sbuf("qT", [128, 4, NTOK], BF16)
    kT = ph.sbuf("kT", [128, 2, NK], BF16)
    vv = ph.sbuf("vv", [128, nkt, 256], BF16)
    y0r = Rot(ph, "y0", 2, [128, 2, 512], F32)
    sqr = Rot(ph, "sq", 2, [128, 2, 512], F32)
    ystage = Rot(ph, "ys", 3, [128, 512], BF16)
    for kvh in range(4):
        ph.dma(qT[:], d["QT"][16 + 4 * kvh:16 + 4 * kvh + 4].rearrange("v p t -> p v t"), writes=["qT"])
        for comp in range(2):
            load_kT(ph, kT[:, comp, :], "kT", d["KBall"][2 * kvh + comp], NK)
        load_v(ph, vv, "vv", d["VBall"], nkt, kvh * 256, 256)
        for gq in range(2):
            head = 2 * kvh + gq
            for (c0, nq, is_ctx) in q_blocks(g):
                y0, y0t = y0r.next()
                for comp in range(2):
                    q_list = [(qT[:, 2 * gq + comp, c0:c0 + nq], "qT")]
                    kts = key_tile_list(g, [(kT[:, comp, :], "kT", 128)], vv, "vv", is_ctx, 256)
                    acc, acct = attend(ph, A, q_list, nq, kts, 2, scale)
                    rc, rct = A.rc.next()
                    ph.dve(lambda e, rc=rc, acc=acc, nq=nq: e.reciprocal(rc[:, 0:nq], acc[2][:, 0:nq]), reads=[acct[2]], writes=[rct])
                    for dc in range(2):
                        if comp == 0:
                            ph.dve(lambda e, dc=dc, y0=y0, acc=acc, rc=rc, nq=nq: e.tensor_tensor(out=y0[:, dc, 0:nq], in0=acc[dc][:, 0:nq], in1=rc[:, 0:nq], op=ALU.mult),
                                   reads=[acct[dc], rct], writes=[y0t])
                        else:
                            sq, sqt = sqr.next() if dc == 0 else (sq, sqt)
                            ph.dve(lambda e, dc=dc, sq=sq, acc=acc, rc=rc, nq=nq: e.tensor_tensor(out=sq[:, dc, 0:nq], in0=acc[dc][:, 0:nq], in1=rc[:, 0:nq], op=ALU.mult),
                                   reads=[acct[dc], rct], writes=[sqt])
                            ph.dve(lambda e, dc=dc, sq=sq, y0=y0, nq=nq: e.scalar_tensor_tensor(out=y0[:, dc, 0:nq], in0=sq[:, dc, 0:nq], scalar=nlam[:, 0:1], in1=y0[:, dc, 0:nq],
                                                                                          op0=ALU.mult, op1=ALU.add),
                                   reads=[sqt, "nlam", y0t], writes=[y0t])
                ph.pool(lambda e, sq=sq, y0=y0, nq=nq: e.tensor_tensor(out=sq[:, :, 0:nq], in0=y0[:, :, 0:nq], in1=y0[:, :, 0:nq], op=ALU.mult),
                        reads=[y0t], writes=[sqt])
                S, St = A.S.next()
                for dc in range(2):
                    ph.pe(lambda e, dc=dc, S=S, sq=sq, nq=nq: e.matmul(S[:, 0:nq], onesf[:], sq[:, dc, 0:nq], start=(dc == 0), stop=(dc == 1)),
                          reads=[sqt, "onesf"], writes=[St])
                rc, rct = A.rc.next()
                ph.dve(lambda e, rc=rc, S=S, nq=nq: e.tensor_scalar(rc[:, 0:nq], S[:, 0:nq], 1.0 / 256, EPS, op0=ALU.mult, op1=ALU.add), reads=[St], writes=[rct])
                ph.act(lambda e, rc=rc, nq=nq: e.activation(out=rc[:, 0:nq], in_=rc[:, 0:nq], func=AF.Sqrt), reads=[rct], writes=[rct])
                ph.dve(lambda e, rc=rc, nq=nq: e.reciprocal(rc[:, 0:nq], rc[:, 0:nq]), reads=[rct], writes=[rct])
                for dc in range(2):
                    ys, yst = ystage.next()
                    ph.dve(lambda e, dc=dc, ys=ys, y0=y0, rc=rc, nq=nq: e.scalar_tensor_tensor(out=ys[:, 0:nq], in0=y0[:, dc, 0:nq], scalar=gsub[:, dc:dc + 1], in1=rc[:, 0:nq],
                                                                                           op0=ALU.mult, op1=ALU.mult),
                           reads=[y0t, "gsub", rct], writes=[yst])
                    ph.dma(d["YT"][16 + 2 * head + dc, :, c0:c0 + nq], ys[:, 0:nq], reads=[yst], queue="sp")
    return ph.finish()


def build_att_c(nc, g, d, l):
    ph = Phase(nc, f"ac{l}")
    NTOK, NK = g.NTOK, g.NK
    nkt = NK // 128
    A = AttRes(ph, 512, 1)
    scale = 128 ** -0.5
    qT = ph.sbuf("qT", [128, 4, NTOK], BF16)
    kT = ph.sbuf("kT", [128, NK], BF16)
    vv = ph.sbuf("vv", [128, nkt, 128], BF16)
    ystage = Rot(ph, "ys", 3, [128, 512], BF16)
    for kvh in range(4):
        ph.dma(qT[:], d["QT"][4 * kvh:4 * kvh + 4].rearrange("v p t -> p v t"), writes=["qT"])
        load_kT(ph, kT, "kT", d["KCall"][kvh], NK)
        load_v(ph, vv, "vv", d["VCall"], nkt, kvh * 128, 128)
        for gq in range(4):
            head = 4 * kvh + gq
            for (c0, nq, is_ctx) in q_blocks(g):
                q_list = [(qT[:, gq, c0:c0 + nq], "qT")]
                kts = key_tile_list(g, [(kT, "kT", 128)], vv, "vv", is_ctx, 128)
                acc, acct = attend(ph, A, q_list, nq, kts, 1, scale)
                finish_simple(ph, A, acc, acct, nq, 1, [d["YT"][head, :, c0:c0 + nq]], ystage)
    return ph.finish()


def build_att_d(nc, g, d, l):
    ph = Phase(nc, f"ad{l}")
    NTOK, NK = g.NTOK, g.NK
    nkt = NK // 128
    A = AttRes(ph, 512, 1)
    scale = 192 ** -0.5
    q0 = ph.sbuf("q0", [128, NTOK], BF16)
    q1 = ph.sbuf("q1", [64, NTOK], BF16)
    k0 = ph.sbuf("k0", [128, NK], BF16)
    k1 = ph.sbuf("k1", [64, NK], BF16)
    vv = ph.sbuf("vv", [128, nkt, 128], BF16)
    ystage = Rot(ph, "ys", 3, [128, 512], BF16)
    for h in range(16):
        ph.dma(q0[:], d["QDT"][h, 0:128, :], writes=["q0"])
        ph.dma(q1[:], d["QDT"][h, 128:192, :], writes=["q1"])
        load_kT(ph, k0, "k0", d["KDall"][h, 0:128, :], NK)
        load_kT(ph, k1, "k1", d["KDall"][h, 128:192, :], NK, parts=64)
        load_v(ph, vv, "vv", d["VDall"], nkt, h * 128, 128)
        for (c0, nq, is_ctx) in q_blocks(g):
            q_list = [(q0[:, c0:c0 + nq], "q0"), (q1[:, c0:c0 + nq], "q1")]
            kts = key_tile_list(g, [(k0, "k0", 128), (k1, "k1", 64)], vv, "vv", is_ctx, 128)
            acc, acct = attend(ph, A, q_list, nq, kts, 1, scale)
            finish_simple(ph, A, acc, acct, nq, 1, [d["YT"][16 + h, :, c0:c0 + nq]], ystage)
    return ph.finish()


def build_mlaup(nc, g, d, l):
    ph = Phase(nc, f"mu{l}")
    NTOK = g.NTOK
    ident, ident_t = make_identity(ph, "ident", 128, BF16)
    wq = ph.sbuf("wq", [128, 12, 3072], BF16)
    for c0 in range(0, 3072, 768):
        load_w_chunk(ph, wq[:, :, c0:c0 + 768], "wq", d["wq_up"], c0, 768, kchunks=12)
    wkv = ph.sbuf("wkv", [128, 4, 4096], BF16)
    for c0 in range(0, 4096, 1024):
        load_w_chunk(ph, wkv[:, :, c0:c0 + 1024], "wkv", d["wkv_up"], c0, 1024, kchunks=4)
    gq = ph.sbuf("gq", [128, 192], F32)
    ph.dma(gq[:], d["d_qn"].to_broadcast([128, 192]), writes=["gq"])
    gk = ph.sbuf("gk", [128, 192], F32)
    ph.dma(gk[:], d["d_kn"].to_broadcast([128, 192]), writes=["gk"])
    qa_r = Rot(ph, "qa", 2, [128, 12, 128], BF16)
    kva_r = Rot(ph, "kva", 2, [128, 4, 128], BF16)
    st_r = Rot(ph, "stat", 2, [128, 4], F32)
    kr_r = Rot(ph, "kr", 2, [128, 64], F32)
    cs_r = Rot(ph, "csr", 2, [128, 2, 64], F32)
    pps = Rot(ph, "pps", 3, [128, 512], F32, psum=True)
    tps = Rot(ph, "tps", 2, [128, 4, 128], BF16, psum=True)
    p_r = Rot(ph, "p", 2, [128, 512], F32)
    sq_r = Rot(ph, "sq", 2, [128, 512], F32)
    ss_r = Rot(ph, "ss", 2, [128, 2], F32)
    y_r = Rot(ph, "y", 2, [128, 2, 192], F32)
    t1_r = Rot(ph, "t1", 2, [128, 2, 64], F32)
    t2_r = Rot(ph, "t2", 2, [128, 2, 64], F32)
    yb_r = Rot(ph, "yb", 2, [128, 2, 192], BF16)
    vb_r = Rot(ph, "vb", 2, [128, 2, 128], BF16)
    so_r = Rot(ph, "so", 2, [128, 4, 128], BF16)
    jk = ph.sbuf("jk", [128, 64], F32)

    def rstd_inplace(ap, tok, n):
        ph.dve(lambda e: e.tensor_scalar(ap, ap, 1.0 / n, EPS, op0=ALU.mult, op1=ALU.add), reads=[tok], writes=[tok])
        ph.act(lambda e: e.activation(out=ap, in_=ap, func=AF.Sqrt), reads=[tok], writes=[tok])
        ph.dve(lambda e: e.reciprocal(ap, ap), reads=[tok], writes=[tok])

    def rope_tail(y, yt, cst, cstt):
        t1, t1t = t1_r.next()
        t2, t2t = t2_r.next()
        ph.pool(lambda e: e.tensor_tensor(out=t1[:], in0=y[:, :, 128:192], in1=cst[:, 0:1, :].to_broadcast([128, 2, 64]), op=ALU.mult),
                reads=[yt, cstt], writes=[t1t])
        yv = y[:, :, 128:192].rearrange("p h (a s f) -> p h a s f", a=2, s=2)
        t2v = t2[:].rearrange("p h (a s f) -> p h a s f", a=2, s=2)
        snv = cst[:, 1, :].rearrange("p (a s f) -> p a s f", a=2, s=2)
        for s_ in range(2):
            ph.dve(lambda e, s_=s_: e.tensor_tensor(out=t2v[:, :, :, s_, :], in0=yv[:, :, :, 1 - s_, :],
                                                   in1=snv[:, :, s_, :].unsqueeze(1).to_broadcast([128, 2, 2, 16]), op=ALU.mult),
                   reads=[yt, cstt, t2t], writes=[t2t])
        ph.pool(lambda e: e.tensor_tensor(out=y[:, :, 128:192], in0=t1[:], in1=t2[:], op=ALU.add), reads=[t1t, t2t, yt], writes=[yt])

    def emit_T(yb, ybt, dest, h0, t):
        tp, tpt = tps.next()
        for hh in range(2):
            ph.pe(lambda e, hh=hh: e.transpose(tp[:, 2 * hh, :], yb[:, hh, 0:128], ident[:]), reads=[ybt, ident_t], writes=[tpt])
            ph.pe(lambda e, hh=hh: e.transpose(tp[0:64, 2 * hh + 1, :], yb[:, hh, 128:192], ident[:]), reads=[ybt, ident_t], writes=[tpt])
        so, sot = so_r.next()
        sov = so[:].rearrange("p (h c) t -> p h c t", c=2)
        tpv = tp[:].rearrange("p (h c) t -> p h c t", c=2)
        ph.act(lambda e: e.activation(out=sov[:, :, 0, :], in_=tpv[:, :, 0, :], func=AF.Copy), reads=[tpt], writes=[sot])
        ph.dve(lambda e: e.tensor_copy(sov[0:64, :, 1, :], tpv[0:64, :, 1, :]), reads=[tpt, sot], writes=[sot])
        for hh in range(2):
            ph.dma(dest[h0 + hh, 0:128, t * 128:(t + 1) * 128], so[:, 2 * hh, :], reads=[sot], queue="sp")
            ph.dma(dest[h0 + hh, 128:192, t * 128:(t + 1) * 128], so[0:64, 2 * hh + 1, :], reads=[sot], queue="sp")

    for t in range(g.NT):
        is_lat = t >= g.NCT
        rows = slice(t * 128, (t + 1) * 128)
        qa, qat = qa_r.next()
        ph.dma(qa[:], d["qaT"][:, :, rows].rearrange("k p t -> p k t"), writes=[qat])
        kva, kvat = kva_r.next()
        ph.dma(kva[:], d["kvaT"][:, :, rows].rearrange("k p t -> p k t"), writes=[kvat])
        st, stt = st_r.next()
        ph.dma(st[:, 0:1], d["ssq_q"][rows, :], writes=[stt])
        ph.dma(st[:, 1:2], d["ssq_kv"][rows, :], writes=[stt])
        krt, krtt = kr_r.next()
        ph.dma(krt[:], d["kr"][rows, :], writes=[krtt])
        if is_lat:
            cst, cstt = cs_r.next()
            lt = t - g.NCT
            ph.dma(cst[:], d["csr"][lt * 128:(lt + 1) * 128], writes=[cstt])
        rstd_inplace(st[:, 0:1], stt, 1536)
        rstd_inplace(st[:, 1:2], stt, 512)
        ph.act(lambda e, st=st, krt=krt: e.activation(out=jk[:], in_=krt[:], func=AF.Square, accum_out=st[:, 2:3]), reads=[krtt, stt], writes=["jk", stt])
        for hp in range(8):
            pp, ppt = pps.next()
            for k in range(12):
                ph.pe(lambda e, pp=pp, k=k, qa=qa, hp=hp: e.matmul(pp[:, 0:384], qa[:, k, :], wq[:, k, hp * 384:(hp + 1) * 384], start=(k == 0), stop=(k == 11)),
                      reads=[qat, "wq"], writes=[ppt])
            p, pt = p_r.next()
            ph.act(lambda e, p=p, pp=pp, st=st: e.activation(out=p[:, 0:384], in_=pp[:, 0:384], func=AF.Copy, scale=st[:, 0:1]), reads=[ppt, stt], writes=[pt])
            sq, sqt = sq_r.next()
            ph.dve(lambda e, sq=sq, p=p: e.tensor_tensor(out=sq[:, 0:384], in0=p[:, 0:384], in1=p[:, 0:384], op=ALU.mult), reads=[pt], writes=[sqt])
            ss, sst = ss_r.next()
            ph.dve(lambda e, ss=ss, sq=sq: e.tensor_reduce(out=ss[:], in_=sq[:, 0:384].rearrange("p (h d) -> p h d", d=192), axis=AX.X, op=ALU.add), reads=[sqt], writes=[sst])
            rstd_inplace(ss[:], sst, 192)
            y, yt = y_r.next()
            ph.dve(lambda e, y=y, p=p, ss=ss: e.tensor_tensor(out=y[:], in0=p[:, 0:384].rearrange("p (h d) -> p h d", d=192),
                                                            in1=ss[:].unsqueeze(2).to_broadcast([128, 2, 192]), op=ALU.mult), reads=[pt, sst], writes=[yt])
            ph.pool(lambda e, y=y: e.tensor_tensor(out=y[:], in0=y[:], in1=gq[:].unsqueeze(1).to_broadcast([128, 2, 192]), op=ALU.mult), reads=[yt, "gq"], writes=[yt])
            if is_lat:
                rope_tail(y, yt, cst, cstt)
            yb, ybt = yb_r.next()
            ph.pool(lambda e, yb=yb, y=y: e.tensor_copy(yb[:], y[:]), reads=[yt], writes=[ybt])
            emit_T(yb, ybt, d["QDT"], 2 * hp, t)
        for hp in range(8):
            pp, ppt = pps.next()
            for k in range(4):
                ph.pe(lambda e, pp=pp, k=k, kva=kva, hp=hp: e.matmul(pp[:], kva[:, k, :], wkv[:, k, hp * 512:(hp + 1) * 512], start=(k == 0), stop=(k == 3)),
                      reads=[kvat, "wkv"], writes=[ppt])
            p, pt = p_r.next()
            ph.act(lambda e, p=p, pp=pp, st=st: e.activation(out=p[:], in_=pp[:], func=AF.Copy, scale=st[:, 1:2]), reads=[ppt, stt], writes=[pt])
            pv = p[:].rearrange("p (h c d) -> p h c d", h=2, c=2)
            vb, vbt = vb_r.next()
            ph.pool(lambda e, vb=vb, pv=pv: e.tensor_copy(vb[:], pv[:, :, 1, :]), reads=[pt], writes=[vbt])
            ph.dma(d["VD"][rows, hp * 256:(hp + 1) * 256], vb[:].rearrange("p h d -> p (h d)"), reads=[vbt], queue="sp")
            sq, sqt = sq_r.next()
            ph.dve(lambda e, sq=sq, pv=pv: e.tensor_tensor(out=sq[:, 0:256].rearrange("p (h d) -> p h d", h=2), in0=pv[:, :, 0, :], in1=pv[:, :, 0, :], op=ALU.mult), reads=[pt], writes=[sqt])
            ss, sst = ss_r.next()
            ph.dve(lambda e, ss=ss, sq=sq: e.tensor_reduce(out=ss[:], in_=sq[:, 0:256].rearrange("p (h d) -> p h d", h=2), axis=AX.X, op=ALU.add), reads=[sqt], writes=[sst])
            ph.dve(lambda e, ss=ss, st=st: e.tensor_scalar(ss[:], ss[:], st[:, 2:3], None, op0=ALU.add), reads=[sst, stt], writes=[sst])
            rstd_inplace(ss[:], sst, 192)
            y, yt = y_r.next()
            ph.dve(lambda e, y=y, pv=pv, ss=ss: e.tensor_tensor(out=y[:, :, 0:128], in0=pv[:, :, 0, :], in1=ss[:].unsqueeze(2).to_broadcast([128, 2, 128]), op=ALU.mult),
                   reads=[pt, sst], writes=[yt])
            for hh in range(2):
                ph.dve(lambda e, y=y, krt=krt, ss=ss, hh=hh: e.tensor_scalar(y[:, hh, 128:192], krt[:], ss[:, hh:hh + 1], None, op0=ALU.mult),
                       reads=[krtt, sst, yt], writes=[yt])
            ph.pool(lambda e, y=y: e.tensor_tensor(out=y[:], in0=y[:], in1=gk[:].unsqueeze(1).to_broadcast([128, 2, 192]), op=ALU.mult), reads=[yt, "gk"], writes=[yt])
            if is_lat:
                rope_tail(y, yt, cst, cstt)
            yb, ybt = yb_r.next()
            ph.pool(lambda e, yb=yb, y=y: e.tensor_copy(yb[:], y[:]), reads=[yt], writes=[ybt])
            emit_T(yb, ybt, d["KDT"], 2 * hp, t)
    return ph.finish()


def build_out(nc, g, d, l):
    ph = Phase(nc, f"ot{l}")
    yT = ph.sbuf("yT", [128, 32, 512], BF16)
    m2 = ph.sbuf("m2", [128, 4096], F32)
    wr = Rot(ph, "w", 2, [128, 32, 512], BF16)
    pps = Rot(ph, "pps", 3, [128, 512], F32, psum=True)
    xr = Rot(ph, "x", 3, [128, 512], F32)
    for grp in tile_groups(g):
        j = 1 if grp[0] < g.NCT else 0
        ph.dma(m2[:], d["mod"][l][j:j + 1, 8192:12288].to_broadcast([128, 4096]), writes=["m2"])
        c0t = grp[0] * 128
        nt_ = len(grp) * 128
        for k0 in range(0, 32, 8):
            ph.dma(yT[:, k0:k0 + 8, 0:nt_], d["YT"][k0:k0 + 8, :, c0t:c0t + nt_].rearrange("k p t -> p k t"), writes=["yT"])
        for c in range(8):
            w, wt_ = wr.next()
            load_w_chunk(ph, w, wt_, d["w_out"], c * 512, 512)
            for ti, t in enumerate(grp):
                pp, ppt = pps.next()
                for k in range(32):
                    ph.pe(lambda e, pp=pp, k=k, w=w, ti=ti: e.matmul(pp[:], yT[:, k, ti * 128:(ti + 1) * 128], w[:, k, :], start=(k == 0), stop=(k == 31)),
                          reads=["yT", wt_], writes=[ppt])
                xt, xtt = xr.next()
                ph.dma(xt[:], d["x"][t * 128:(t + 1) * 128, c * 512:(c + 1) * 512], writes=[xtt])
                tmp, tmpt = xr.next()
                ph.dve(lambda e, tmp=tmp, pp=pp, c=c: e.tensor_tensor(out=tmp[:], in0=pp[:], in1=m2[:, c * 512:(c + 1) * 512], op=ALU.mult), reads=[ppt, "m2"], writes=[tmpt])
                ph.pool(lambda e, tmp=tmp, xt=xt: e.tensor_tensor(out=tmp[:], in0=tmp[:], in1=xt[:], op=ALU.add), reads=[tmpt, xtt], writes=[tmpt])
                ph.dma(d["xo"][t * 128:(t + 1) * 128, c * 512:(c + 1) * 512], tmp[:], reads=[tmpt], queue="act")
    return ph.finish()


def build_moe(nc, g, d, l, max_groups=4):
    need_ctx = l < g.DEPTH - 1
    groups = tile_groups(g)
    if not need_ctx:
        groups = groups[1:]
    n = 0
    max_groups = DBG.get("moe_max_groups", max_groups)
    for ci, i0 in enumerate(range(0, len(groups), max_groups)):
        n += _moe_phase(nc, g, d, l, groups[i0:i0 + max_groups], f"me{l}_{ci}", passthrough=(not need_ctx and ci == 0))
    return n


def _moe_phase(nc, g, d, l, groups, name, passthrough):
    ph = Phase(nc, name)
    NE = g.NE
    ident, ident_t = make_identity(ph, "ident", 128, BF16)
    identf, identf_t = "ident_f_dummy", None
    idf = ph.sbuf("idf2", [128, 128], F32)
    ph.pool(lambda e: e.memset(idf[:], 1.0), writes=["idf2"])
    ph.pool(lambda e: e.affine_select(out=idf[:], in_=idf[:], pattern=[[-1, 128]], compare_op=ALU.is_equal, fill=0.0, base=0, channel_multiplier=1),
            reads=["idf2"], writes=["idf2"])
    gmT = ph.sbuf("gmT", [128, 32], F32)
    shT = ph.sbuf("shT", [128, 32], F32)
    gnT = ph.sbuf("gnT", [128, 32], F32)
    ph.dma(gnT[:], d["norm_ffn"][l:l + 1, :].rearrange("o (k p) -> p (o k)", p=128), writes=["gnT"], allow_slow_non_contiguous=True)
    rw = ph.sbuf("rw", [128, 32, NE], BF16)
    ph.dma(rw[:], d["router_w"].rearrange("(k p) n -> p k n", p=128), writes=["rw"], queue="pool")
    rb = ph.sbuf("rb", [128, NE], F32)
    ph.dma(rb[:], d["router_b"].to_broadcast([128, NE]), writes=["rb"])
    bgu = ph.sbuf("bgu", [128, NE, 6], F32)
    for e_ in range(NE):
        ph.dma(bgu[:, e_, :], d["b_gu"][e_:e_ + 1, :].rearrange("o (c p) -> p (o c)", p=128), writes=["bgu"], allow_slow_non_contiguous=True)
    hT = ph.sbuf("hT", [128, 32, 512], BF16)
    yacc = [ph.sbuf(f"yacc{i}", [128, 4096], F32) for i in range(4)]
    gates = ph.sbuf("gates", [128, 4, NE], F32)
    gT = ph.sbuf("gT", [NE, 512], F32)
    xr = Rot(ph, "x", 2, [128, 2048], F32)
    xnr = Rot(ph, "xn", 1, [128, 4096], BF16)
    ssr = Rot(ph, "ss", 2, [128, 2], F32)
    tpr = Rot(ph, "tp", 2, [128, 4, 128], BF16, psum=True)
    wgr = Rot(ph, "wg", 2, [128, 32, 256], BF16)
    wdr = Rot(ph, "wd", 2, [128, 3, 1024], BF16)
    gps = Rot(ph, "gps", 2, [128, 512], F32, psum=True)
    dps = Rot(ph, "dps", 3, [128, 512], F32, psum=True)
    glu = ph.sbuf("glu", [128, 3, 512], F32)
    lin = ph.sbuf("lin", [128, 3, 512], F32)
    actr = Rot(ph, "act", 2, [128, 3, 512], BF16)
    lg = ph.sbuf("lg", [128, NE], F32)
    mx8 = ph.sbuf("mx8", [128, 8], F32)
    msk = ph.sbuf("msk", [128, NE], F32)
    nmx = ph.sbuf("nmx", [128, 1], F32)
    sm = ph.sbuf("sm", [128, 1], F32)
    bdr = Rot(ph, "bd", 2, [NE, 512], F32)
    m5r = Rot(ph, "m5", 2, [128, 512], F32)
    xcr = Rot(ph, "xc", 2, [128, 512], F32)
    ev = [0]
    for grp in groups:
        j = 1 if grp[0] < g.NCT else 0
        T = len(grp) * 128
        ph.dma(gmT[:], d["mod"][l][j:j + 1, 4 * 4096:5 * 4096].rearrange("o (k p) -> p (o k)", p=128), writes=["gmT"], allow_slow_non_contiguous=True)
        ph.dma(shT[:], d["mod"][l][j:j + 1, 3 * 4096:4 * 4096].rearrange("o (k p) -> p (o k)", p=128), writes=["shT"], allow_slow_non_contiguous=True)
        ph.dve(lambda e: e.scalar_tensor_tensor(out=gmT[:], in0=gmT[:], scalar=1.0, in1=gnT[:], op0=ALU.add, op1=ALU.mult), reads=["gmT", "gnT"], writes=["gmT"])
        for ti, t in enumerate(grp):
            xn, xnt = xnr.next()
            ss, sst = ssr.next()
            xh = []
            for hf in range(2):
                xt, xtt = xr.next()
                xh.append((xt, xtt))
                ph.dma(xt[:], d["x"][t * 128:(t + 1) * 128, hf * 2048:(hf + 1) * 2048], writes=[xtt])
                ph.act(lambda e, xn=xn, xt=xt, ss=ss, hf=hf: e.activation(out=xn[:, hf * 2048:(hf + 1) * 2048], in_=xt[:], func=AF.Square, accum_out=ss[:, hf:hf + 1]),
                       reads=[xtt, sst], writes=[xnt, sst])
            ph.dve(lambda e, ss=ss: e.tensor_tensor(out=ss[:, 0:1], in0=ss[:, 0:1], in1=ss[:, 1:2], op=ALU.add), reads=[sst], writes=[sst])
            ph.dve(lambda e, ss=ss: e.tensor_scalar(ss[:, 0:1], ss[:, 0:1], 1.0 / 4096, EPS, op0=ALU.mult, op1=ALU.add), reads=[sst], writes=[sst])
            ph.act(lambda e, ss=ss: e.activation(out=ss[:, 0:1], in_=ss[:, 0:1], func=AF.Sqrt), reads=[sst], writes=[sst])
            ph.dve(lambda e, ss=ss: e.reciprocal(ss[:, 0:1], ss[:, 0:1]), reads=[sst], writes=[sst])
            for hf in range(2):
                xt, xtt = xh[hf]
                ph.act(lambda e, xn=xn, xt=xt, ss=ss, hf=hf: e.activation(out=xn[:, hf * 2048:(hf + 1) * 2048], in_=xt[:], func=AF.Copy, scale=ss[:, 0:1]),
                       reads=[xtt, sst, xnt], writes=[xnt])
            for k4 in range(8):
                tp, tpt = tpr.next()
                for i in range(4):
                    ph.pe(lambda e, tp=tp, xn=xn, i=i, k4=k4: e.transpose(tp[:, i, :], xn[:, (k4 * 4 + i) * 128:(k4 * 4 + i + 1) * 128], ident[:]),
                          reads=[xnt, ident_t], writes=[tpt])
                for i in range(4):
                    kc = k4 * 4 + i
                    ph.dve(lambda e, tp=tp, i=i, kc=kc, ti=ti: e.tensor_scalar(hT[:, kc, ti * 128:(ti + 1) * 128], tp[:, i, :], gmT[:, kc:kc + 1], shT[:, kc:kc + 1],
                                                                            op0=ALU.mult, op1=ALU.add),
                           reads=[tpt, "gmT", "shT"], writes=["hT"])
        for ti, t in enumerate(grp):
            lp, lpt = gps.next()
            for k in range(32):
                ph.pe(lambda e, lp=lp, k=k, ti=ti: e.matmul(lp[:, 0:NE], hT[:, k, ti * 128:(ti + 1) * 128], rw[:, k, :], start=(k == 0), stop=(k == 31)),
                      reads=["hT", "rw"], writes=[lpt])
            ph.dve(lambda e, lp=lp: e.tensor_tensor(out=lg[:], in0=lp[:, 0:NE], in1=rb[:], op=ALU.add), reads=[lpt, "rb"], writes=["lg"])
            ph.dve(lambda e: e.max(out=mx8[:], in_=lg[:]), reads=["lg"], writes=["mx8"])
            ph.dve(lambda e: e.tensor_scalar(msk[:], lg[:], mx8[:, 3:4], None, op0=ALU.is_ge), reads=["lg", "mx8"], writes=["msk"])
            ph.dve(lambda e: e.tensor_scalar(nmx[:], mx8[:, 0:1], -1.0, None, op0=ALU.mult), reads=["mx8"], writes=["nmx"])
            ph.act(lambda e: e.activation(out=lg[:], in_=lg[:], func=AF.Exp, bias=nmx[:, 0:1], scale=1.0), reads=["lg", "nmx"], writes=["lg"])
            ph.dve(lambda e: e.tensor_tensor(out=lg[:], in0=lg[:], in1=msk[:], op=ALU.mult), reads=["lg", "msk"], writes=["lg"])
            ph.dve(lambda e: e.tensor_reduce(out=sm[:], in_=lg[:], axis=AX.X, op=ALU.add), reads=["lg"], writes=["sm"])
            ph.dve(lambda e: e.reciprocal(sm[:], sm[:]), reads=["sm"], writes=["sm"])
            ph.dve(lambda e, ti=ti: e.tensor_scalar(gates[:, ti, :], lg[:], sm[:, 0:1], None, op0=ALU.mult), reads=["lg", "sm"], writes=["gates"])
            gp, gpt = gps.next()
            ph.pe(lambda e, gp=gp, ti=ti: e.transpose(gp[0:NE, 0:128], gates[:, ti, :], idf[:]), reads=["gates", "idf2"], writes=[gpt])
            ph.dve(lambda e, gp=gp, ti=ti: e.tensor_copy(gT[:, ti * 128:(ti + 1) * 128], gp[0:NE, 0:128]), reads=[gpt], writes=["gT"])
        for c in range(8):
            bd, bdt = bdr.next()
            ph.dma(bd[:], d["b_down"][:, c * 512:(c + 1) * 512], writes=[bdt])
            for ti, t in enumerate(grp):
                dp, dpt = dps.next()
                ph.pe(lambda e, dp=dp, bd=bd, ti=ti: e.matmul(dp[:], gT[:, ti * 128:(ti + 1) * 128], bd[:], start=True, stop=True), reads=["gT", bdt], writes=[dpt])
                ev[0] += 1
                if ev[0] % 2:
                    ph.act(lambda e, dp=dp, ti=ti, c=c: e.activation(out=yacc[ti][:, c * 512:(c + 1) * 512], in_=dp[:], func=AF.Copy), reads=[dpt], writes=[("yacc", ti)])
                else:
                    ph.dve(lambda e, dp=dp, ti=ti, c=c: e.tensor_copy(yacc[ti][:, c * 512:(c + 1) * 512], dp[:]), reads=[dpt], writes=[("yacc", ti)])
        for ex in range(NE):
            for pi in range(3):
                w, wt_ = wgr.next()
                load_w_chunk(ph, w, wt_, d["w_gu"][ex], pi * 256, 256)
                for fl in range(2):
                    fc = 2 * pi + fl
                    gp, gpt = gps.next()
                    for k in range(32):
                        ph.pe(lambda e, gp=gp, k=k, w=w, fl=fl, T=T: e.matmul(gp[:, 0:T], w[:, k, fl * 128:(fl + 1) * 128], hT[:, k, 0:T], start=(k == 0), stop=(k == 31)),
                              reads=["hT", wt_], writes=[gpt])
                    if fc < 3:
                        ph.dve(lambda e, gp=gp, fc=fc, ex=ex, T=T: e.tensor_scalar(glu[:, fc, 0:T], gp[:, 0:T], bgu[:, ex, fc:fc + 1], 7.0, op0=ALU.add, op1=ALU.min),
                               reads=[gpt, "bgu"], writes=["glu"])
                    else:
                        ph.dve(lambda e, gp=gp, fc=fc, ex=ex, T=T: e.tensor_scalar(lin[:, fc - 3, 0:T], gp[:, 0:T], bgu[:, ex, fc:fc + 1], 7.0, op0=ALU.add, op1=ALU.min),
                               reads=[gpt, "bgu"], writes=["lin"])
            ph.pool(lambda e, T=T: e.tensor_scalar(lin[:, :, 0:T], lin[:, :, 0:T], -7.0, 1.0, op0=ALU.max, op1=ALU.add), reads=["lin"], writes=["lin"])
            ph.pool(lambda e, T=T: e.tensor_tensor(out=lin[:, :, 0:T], in0=lin[:, :, 0:T], in1=glu[:, :, 0:T], op=ALU.mult), reads=["lin", "glu"], writes=["lin"])
            ph.act(lambda e, T=T: e.activation(out=glu[:, :, 0:T], in_=glu[:, :, 0:T], func=AF.Sigmoid, scale=1.702), reads=["glu", "lin"], writes=["glu"])
            at, att = actr.next()
            ph.dve(lambda e, at=at, T=T: e.tensor_tensor(out=at[:, :, 0:T], in0=lin[:, :, 0:T], in1=glu[:, :, 0:T], op=ALU.mult), reads=["lin", "glu"], writes=[att])
            for pi in range(4):
                wd, wdt = wdr.next()
                load_w_chunk(ph, wd, wdt, d["w_down"][ex], pi * 1024, 1024, kchunks=3)
                for ti, t in enumerate(grp):
                    for cc in range(2):
                        dp, dpt = dps.next()
                        for fc in range(3):
                            ph.pe(lambda e, dp=dp, fc=fc, at=at, wd=wd, ti=ti, cc=cc: e.matmul(dp[:], at[:, fc, ti * 128:(ti + 1) * 128], wd[:, fc, cc * 512:(cc + 1) * 512],
                                                                                           start=(fc == 0), stop=(fc == 2)),
                                  reads=[att, wdt], writes=[dpt])
                        c0 = pi * 1024 + cc * 512
                        ph.dve(lambda e, dp=dp, ti=ti, c0=c0, ex=ex: e.scalar_tensor_tensor(out=yacc[ti][:, c0:c0 + 512], in0=dp[:], scalar=gates[:, ti, ex:ex + 1],
                                                                                          in1=yacc[ti][:, c0:c0 + 512], op0=ALU.mult, op1=ALU.add),
                               reads=[dpt, "gates", ("yacc", ti)], writes=[("yacc", ti)])
        for c in range(8):
            m5, m5t = m5r.next()
            ph.dma(m5[:], d["mod"][l][j:j + 1, 5 * 4096 + c * 512:5 * 4096 + (c + 1) * 512].to_broadcast([128, 512]), writes=[m5t])
            for ti, t in enumerate(grp):
                xc, xct = xcr.next()
                ph.dma(xc[:], d["x"][t * 128:(t + 1) * 128, c * 512:(c + 1) * 512], writes=[xct])
                ph.pool(lambda e, ti=ti, c=c, m5=m5: e.tensor_tensor(out=yacc[ti][:, c * 512:(c + 1) * 512], in0=yacc[ti][:, c * 512:(c + 1) * 512], in1=m5[:], op=ALU.mult),
                        reads=[("yacc", ti), m5t], writes=[("yacc", ti)])
                ph.pool(lambda e, ti=ti, c=c, xc=xc: e.tensor_tensor(out=xc[:], in0=yacc[ti][:, c * 512:(c + 1) * 512], in1=xc[:], op=ALU.add),
                        reads=[("yacc", ti), xct], writes=[xct])
                ph.dma(d["xo"][t * 128:(t + 1) * 128, c * 512:(c + 1) * 512], xc[:], reads=[xct], queue="act")
    if passthrough:
        for t in range(g.NCT):
            for hf in range(2):
                xt, xtt = xr.next()
                ph.dma(xt[:], d["x"][t * 128:(t + 1) * 128, hf * 2048:(hf + 1) * 2048], writes=[xtt])
                ph.dma(d["xo"][t * 128:(t + 1) * 128, hf * 2048:(hf + 1) * 2048], xt[:], reads=[xtt], queue="act")
    return ph.finish()


def run_model(inp, g, dbg=None):
    NC, TL, CT, NTOK, NK = g.NC, g.TL, g.CT, g.NTOK, g.NK
    L = g.DEPTH
    f32 = np.float32
    x = np.asarray(inp["x"], f32)[0]
    ctx = np.asarray(inp["ctx"], f32)[0]
    A = lambda k: np.ascontiguousarray(np.asarray(inp[k], f32))
    c2 = np.stack([A("c")[0], A("c_ctx")])
    maps = []
    for r in range(NC):
        t = r * TL + np.arange(TL)
        pos = np.stack([t // 64, t % 64], 1).astype(f32)
        maps.append(dict(c2=c2, adaln_down=A("adaln_down"), adaln_up=A("adaln_up"), adaln_b=A("adaln_b"), pos=pos))
    res = launch(build_pre, g, maps, dict(mod=((L, 2, 24576), F32), cs=((TL, 2, 128), F32), csr=((TL, 2, 64), F32)))
    pre = res.results
    xs = [np.concatenate([ctx, x[r * TL:(r + 1) * TL]], 0) for r in range(NC)]
    norm_mix, norm_ffn = A("norm_mix"), A("norm_ffn")
    for l in range(L):
        j = l // 2
        even = l % 2 == 0
        maps = []
        for r in range(NC):
            m = dict(x=xs[r], mod=pre[r]["mod"], norm_mix=norm_mix, cs=pre[r]["cs"])
            if even:
                m.update(w_in=A("ev_w_in")[j], a_qn=A("ev_a_qn")[j:j + 1], a_kn=A("ev_a_kn")[j:j + 1],
                         b_qn=A("ev_b_qn")[j:j + 1], b_kn=A("ev_b_kn")[j:j + 1])
            else:
                m.update(w_in=A("od_w_in")[j], c_qn=A("od_c_qn")[j:j + 1], c_kn=A("od_c_kn")[j:j + 1],
                         d_qa_norm=A("od_d_qa_norm")[j:j + 1], d_kva_norm=A("od_d_kva_norm")[j:j + 1])
            maps.append(m)
        if even:
            outs = dict(QT=((32, 128, NTOK), BF16), KT=((12, 128, NTOK), BF16), V=((NTOK, 1536), BF16))
        else:
            outs = dict(QT=((16, 128, NTOK), BF16), KT=((4, 128, NTOK), BF16), V=((NTOK, 512), BF16),
                        qaT=((12, 128, NTOK), BF16), kvaT=((4, 128, NTOK), BF16), kr=((NTOK, 64), F32),
                        ssq_q=((NTOK, 1), F32), ssq_kv=((NTOK, 1), F32))
        pj = launch(lambda nc, g_, d: build_proj(nc, g_, d, l), g, maps, outs).results
        if dbg is not None:
            dbg[f"proj{l}"] = pj
        if not even:
            maps = []
            for r in range(NC):
                maps.append(dict(qaT=pj[r]["qaT"], kvaT=pj[r]["kvaT"], kr=pj[r]["kr"], ssq_q=pj[r]["ssq_q"], ssq_kv=pj[r]["ssq_kv"],
                                 wq_up=A("od_d_wq_up")[j], wkv_up=A("od_d_wkv_up")[j], d_qn=A("od_d_qn")[j:j + 1], d_kn=A("od_d_kn")[j:j + 1],
                                 csr=pre[r]["csr"]))
            mu = launch(lambda nc, g_, d: build_mlaup(nc, g_, d, l), g, maps,
                        dict(QDT=((16, 192, NTOK), BF16), KDT=((16, 192, NTOK), BF16), VD=((NTOK, 2048), BF16))).results
            if dbg is not None:
                dbg[f"mlaup{l}"] = mu
        def gather_kt(arrs, sl):
            return np.ascontiguousarray(np.concatenate([arrs[0][sl][..., :CT]] + [a[sl][..., CT:] for a in arrs], axis=-1))

        def gather_v(arrs, c0, c1):
            return np.ascontiguousarray(np.concatenate([arrs[0][:CT, c0:c1]] + [a[CT:, c0:c1] for a in arrs], axis=0))

        maps = []
        if even:
            KBall = gather_kt([p["KT"] for p in pj], slice(4, 12))
            VBall = gather_v([p["V"] for p in pj], 512, 1536)
            for r in range(NC):
                kt, v = pj[r]["KT"], pj[r]["V"]
                zk = np.zeros((4, 128, 128), kt.dtype)
                zv = np.zeros((128, 512), v.dtype)
                kprev = pj[r - 1]["KT"][0:4, :, NTOK - 128:] if r > 0 else zk
                knext = pj[r + 1]["KT"][0:4, :, CT:CT + 128] if r < NC - 1 else zk
                vprev = pj[r - 1]["V"][NTOK - 128:, 0:512] if r > 0 else zv
                vnext = pj[r + 1]["V"][CT:CT + 128, 0:512] if r < NC - 1 else zv
                KAw = np.ascontiguousarray(np.concatenate([kprev, kt[0:4, :, CT:], knext], axis=-1))
                VAw = np.ascontiguousarray(np.concatenate([vprev, v[CT:, 0:512], vnext], axis=0))
                flags = np.zeros((128, 2), f32)
                flags[:, 0] = 1.0 if r > 0 else 0.0
                flags[:, 1] = 1.0 if r < NC - 1 else 0.0
                maps.append(dict(QT=pj[r]["QT"], KT=kt, V=v, KAw=KAw, VAw=VAw, flags=flags, a_sink=A("ev_a_sink")[j:j + 1],
                                 KBall=KBall, VBall=VBall, b_lam=A("ev_b_lam")[j].reshape(1, 512), b_subln=A("ev_b_subln")[j:j + 1]))

            def build_att(nc, g_, d):
                build_att_a(nc, g_, d, l)
                build_att_b(nc, g_, d, l)
        else:
            KCall = gather_kt([p["KT"] for p in pj], slice(0, 4))
            VCall = gather_v([p["V"] for p in pj], 0, 512)
            KDall = gather_kt([p["KDT"] for p in mu], slice(0, 16))
            VDall = gather_v([p["VD"] for p in mu], 0, 2048)
            for r in range(NC):
                maps.append(dict(QT=pj[r]["QT"], KCall=KCall, VCall=VCall, QDT=mu[r]["QDT"], KDall=KDall, VDall=VDall))

            def build_att(nc, g_, d):
                build_att_c(nc, g_, d, l)
                build_att_d(nc, g_, d, l)
        at = launch(build_att, g, maps, dict(YT=((32, 128, NTOK), BF16))).results
        if dbg is not None:
            dbg[f"att{l}"] = at
        w_out = A("ev_w_out")[j] if even else A("od_w_out")[j]
        maps = [dict(YT=at[r]["YT"], w_out=w_out, x=xs[r], mod=pre[r]["mod"]) for r in range(NC)]
        ot = launch(lambda nc, g_, d: build_out(nc, g_, d, l), g, maps, dict(xo=((NTOK, 4096), F32))).results
        xs = [ot[r]["xo"] for r in range(NC)]
        if dbg is not None:
            dbg[f"out{l}"] = [a.copy() for a in xs]
        NCM = 2 if NC % 2 == 0 else NC
        per = NC // NCM
        gm = Geo(TL=TL * per, NE=g.NE, DEPTH=L, NCORES=NCM)
        xs_m = [np.ascontiguousarray(np.concatenate([xs[0][:CT]] + [xs[r][CT:] for r in range(k * per, (k + 1) * per)], 0)) for k in range(NCM)]
        maps = [dict(x=xs_m[k], mod=pre[k]["mod"], norm_ffn=norm_ffn, router_w=A("router_w")[l], router_b=A("router_b")[l:l + 1],
                     w_gu=A("moe_w_gu")[l], b_gu=A("moe_b_gu")[l], w_down=A("moe_w_down")[l], b_down=A("moe_b_down")[l]) for k in range(NCM)]
        me = launch(lambda nc, g_, d: build_moe(nc, g_, d, l), gm, maps, dict(xo=((gm.NTOK, 4096), F32))).results
        ctx_new = me[0]["xo"][:CT]
        xs = [np.ascontiguousarray(np.concatenate([ctx_new, me[r // per]["xo"][CT + (r % per) * TL:CT + (r % per + 1) * TL]], 0)) for r in range(NC)]
        if dbg is not None:
            dbg[f"moe{l}"] = [a.copy() for a in xs]
    out = np.concatenate([a[CT:] for a in xs], 0)[None]
    return out.astype(np.float32)


def kernel(**inputs):
    g = Geo(TL=2048, NE=32, DEPTH=4, NCORES=8)
    return run_model(inputs, g)
```
